# Optimizing a Trainium2 kernel written in Bass

```python
import math
import jax, jax.numpy as jnp
from jax import lax
import numpy as np

D_MODEL = 2048
BATCH = 2
SEQ = 8192
DEPTH = 2

GRID_W = 64
CTX_LEN = 256
HEAD_DIM = 128
ROPE_PAIRS = HEAD_DIM // 4
ROPE_THETA = 10000.0
EPS = 1e-6
NEG_INF = -1e30
Q_BLOCK = 128
A_HEADS = 8
A_KV_HEADS = 2
WINDOW = 128
A_BLOCK = 128
B_HEADS = 4
B_VDIM = 2 * HEAD_DIM
C_HEADS = 8
C_KV_HEADS = 2
A_Q = A_HEADS * HEAD_DIM
A_KV = A_KV_HEADS * HEAD_DIM
B_QK = B_HEADS * 2 * HEAD_DIM
B_V = B_HEADS * B_VDIM
C_Q = C_HEADS * HEAD_DIM
C_KV = C_KV_HEADS * HEAD_DIM
BRANCH_WIDTH = 1024
IN_COLS = A_Q + 2 * A_KV + 2 * B_QK + B_V + C_Q + 2 * C_KV + 3 * D_MODEL
D_FF = 5632
N_EXPERTS = 8
TOP_K = 2
D_FF_EXPERT = 7168
MOE_BLOCK = 256
N_DENSE = (DEPTH + 1) // 2
N_MOE = DEPTH // 2

kernel_name = "hybrid_diffusion_gated_branch_trunk"

f32 = jnp.float32


def rmsnorm(x, g):
    xf = x.astype(f32)
    y = xf * lax.rsqrt(jnp.mean(xf * xf, axis=-1, keepdims=True) + EPS)
    return (y * g.astype(f32)).astype(x.dtype)


def axial_rope(n_tokens):
    rows = n_tokens // GRID_W
    row = jnp.repeat(jnp.arange(rows, dtype=jnp.int32), GRID_W)
    col = jnp.tile(jnp.arange(GRID_W, dtype=jnp.int32), rows)
    inv = ROPE_THETA ** (-jnp.arange(ROPE_PAIRS, dtype=f32) / ROPE_PAIRS)
    ang = jnp.stack([row.astype(f32)[:, None] * inv, col.astype(f32)[:, None] * inv], axis=1)
    return jnp.cos(ang), jnp.sin(ang)


def apply_rope(t, cos, sin):
    xr = t.astype(f32).reshape(t.shape[:-1] + (2, 2, ROPE_PAIRS))
    x1, x2 = xr[..., 0, :], xr[..., 1, :]
    c, s = cos[:, None], sin[:, None]
    out = jnp.stack([x1 * c - x2 * s, x1 * s + x2 * c], axis=-2)
    return out.reshape(t.shape).astype(t.dtype)


def heads(t, n):
    return t.reshape(t.shape[:-1] + (n, HEAD_DIM))


def project(h, w_in, qk_g, rope):
    sizes = [A_Q, A_KV, A_KV, B_QK, B_QK, B_V, C_Q, C_KV, C_KV, D_MODEL, D_MODEL, D_MODEL]
    idx = [int(v) for v in np.cumsum(sizes)[:-1]]
    aq, ak, av, bq, bk, bv, cq, ck, cv, ga, gb, gc = jnp.split(h @ w_in, idx, axis=-1)
    B_, T = h.shape[0], h.shape[1]
    qks = [rmsnorm(heads(aq, A_HEADS), qk_g[0]), rmsnorm(heads(ak, A_KV_HEADS), qk_g[1]),
           rmsnorm(heads(bq, 2 * B_HEADS), qk_g[2]), rmsnorm(heads(bk, 2 * B_HEADS), qk_g[3]),
           rmsnorm(heads(cq, C_HEADS), qk_g[4]), rmsnorm(heads(ck, C_KV_HEADS), qk_g[5])]
    if rope is not None:
        qks = [apply_rope(t, rope[0], rope[1]) for t in qks]
    aq, ak, bq, bk, cq, ck = qks
    bq = bq.reshape(B_, T, B_HEADS, 2, HEAD_DIM)
    bk = bk.reshape(B_, T, B_HEADS, 2, HEAD_DIM)
    bv = bv.reshape(B_, T, B_HEADS, B_VDIM)
    gates = (jax.nn.sigmoid(ga), jax.nn.sigmoid(gb), jax.nn.sigmoid(gc))
    return aq, ak, heads(av, A_KV_HEADS), bq, bk, bv, cq, ck, heads(cv, C_KV_HEADS), gates


def gqa(q, k, v, sink=None):
    B_, T, Hq, dh = q.shape
    Hkv = k.shape[2]
    G = Hq // Hkv
    qg = q.reshape(B_, T, Hkv, G, dh)
    s = jnp.einsum('btkgd,blkd->bkgtl', qg, k).astype(f32) * dh ** -0.5
    if sink is not None:
        s_sink = jnp.broadcast_to(sink.astype(f32).reshape(Hkv, G, 1, 1), s.shape[:-1] + (1,))
        s = jnp.concatenate([s, s_sink], axis=-1)
    p = jax.nn.softmax(s, axis=-1)
    if sink is not None:
        p = p[..., :-1]
    o = jnp.einsum('bkgtl,blkd->btkgd', p.astype(v.dtype), v)
    return o.reshape(B_, T, Hq * dh)


def window_gqa(q, k, v, kc, vc, sink):
    B_, S, Hq, dh = q.shape
    Hkv = k.shape[2]
    G = Hq // Hkv
    nb = S // A_BLOCK
    C = kc.shape[1]
    scale = dh ** -0.5
    qb = q.reshape(B_, nb, A_BLOCK, Hkv, G, dh)

    def band(t):
        tp = jnp.pad(t, ((0, 0), (A_BLOCK, A_BLOCK), (0, 0), (0, 0)))
        tp = tp.reshape(B_, nb + 2, A_BLOCK, Hkv, dh)
        return jnp.concatenate([tp[:, :-2], tp[:, 1:-1], tp[:, 2:]], axis=2)

    kb, vb = band(k), band(v)
    s_ctx = jnp.einsum('bnqkgd,bckd->bnkgqc', qb, kc).astype(f32) * scale
    s_loc = jnp.einsum('bnqkgd,bnskd->bnkgqs', qb, kb).astype(f32) * scale
    blk = jnp.arange(nb, dtype=jnp.int32)[:, None]
    qpos = blk * A_BLOCK + jnp.arange(A_BLOCK, dtype=jnp.int32)[None, :]
    kpos = (blk - 1) * A_BLOCK + jnp.arange(3 * A_BLOCK, dtype=jnp.int32)[None, :]
    valid = ((jnp.abs(qpos[:, :, None] - kpos[:, None, :]) <= WINDOW)
             & (kpos >= 0)[:, None, :] & (kpos < S)[:, None, :])
    s_loc = jnp.where(valid[None, :, None, None], s_loc, NEG_INF)
    s_sink = jnp.broadcast_to(sink.astype(f32).reshape(1, 1, Hkv, G, 1, 1), s_loc.shape[:-1] + (1,))
    p = jax.nn.softmax(jnp.concatenate([s_ctx, s_loc, s_sink], axis=-1), axis=-1)
    p_ctx = p[..., :C].astype(v.dtype)
    p_loc = p[..., C:C + 3 * A_BLOCK].astype(v.dtype)
    o = (jnp.einsum('bnkgqc,bckd->bnqkgd', p_ctx, vc)
         + jnp.einsum('bnkgqs,bnskd->bnqkgd', p_loc, vb))
    return o.reshape(B_, S, Hq * dh)


def diff_attn(q, k, v, lam):
    s = jnp.einsum('bthcd,blhcd->bhctl', q, k).astype(f32) * HEAD_DIM ** -0.5
    p = jax.nn.softmax(s, axis=-1)
    a = p[:, :, 0] - lam * p[:, :, 1]
    return jnp.einsum('bhtl,blhe->bthe', a.astype(v.dtype), v)


def over_query_blocks(fn, q):
    B_, S = q.shape[0], q.shape[1]
    nb = S // Q_BLOCK
    qb = jnp.moveaxis(q.reshape((B_, nb, Q_BLOCK) + q.shape[2:]), 1, 0)
    ob = jnp.moveaxis(lax.map(fn, qb), 0, 1)
    return ob.reshape((B_, S) + ob.shape[3:])


def diff_subln(o, g, lam_init):
    o = rmsnorm(o, g) * (1.0 - lam_init)
    return o.reshape(o.shape[:2] + (B_V,))


def merge(oa, ob, oc, gates, w_branch, w_out):
    ga, gb, gc = gates
    y = ga * (oa @ w_branch[0]) + gb * (ob @ w_branch[1]) + gc * (oc @ w_branch[2])
    return y @ w_out


def mixer(h, hc, w_in, qk_g, a_sink, b_lam, b_subln, w_branch, w_out, lam_init, with_ctx):
    rope = axial_rope(h.shape[1])
    aq, ak, av, bq, bk, bv, cq, ck, cv, gates = project(h, w_in, qk_g, rope)
    aqc, akc, avc, bqc, bkc, bvc, cqc, ckc, cvc, gates_c = project(hc, w_in, qk_g, None)
    lam = (jnp.exp(jnp.sum(b_lam[0].astype(f32) * b_lam[1].astype(f32)))
           - jnp.exp(jnp.sum(b_lam[2].astype(f32) * b_lam[3].astype(f32))) + lam_init)
    oa = window_gqa(aq, ak, av, akc, avc, a_sink)
    bk_all = jnp.concatenate([bkc, bk], axis=1)
    bv_all = jnp.concatenate([bvc, bv], axis=1)
    ob = diff_subln(over_query_blocks(lambda qb: diff_attn(qb, bk_all, bv_all, lam), bq), b_subln, lam_init)
    ck_all = jnp.concatenate([ckc, ck], axis=1)
    cv_all = jnp.concatenate([cvc, cv], axis=1)
    oc = over_query_blocks(lambda qb: gqa(qb, ck_all, cv_all), cq)
    y = merge(oa, ob, oc, gates, w_branch, w_out)
    if not with_ctx:
        return y, None
    oac = gqa(aqc, akc, avc, a_sink)
    obc = diff_subln(diff_attn(bqc, bkc, bvc, lam), b_subln, lam_init)
    occ = gqa(cqc, ckc, cvc)
    yc = merge(oac, obc, occ, gates_c, w_branch, w_out)
    return y, yc


def swiglu(t, w1, w3, w2):
    return (jax.nn.silu(t @ w1) * (t @ w3)) @ w2


def moe_swiglu(h, w_router, w1, w3, w2):
    shp = h.shape
    t = h.reshape(-1, shp[-1])
    n = t.shape[0]
    logits = (t @ w_router).astype(f32)
    top_v, top_i = lax.top_k(logits, TOP_K)
    wts = jax.nn.softmax(top_v, axis=-1).astype(t.dtype)
    flat_e = top_i.reshape(-1).astype(jnp.int32)
    n_slots = n * TOP_K
    order = jnp.argsort(flat_e)
    sorted_e = flat_e[order]
    counts = jnp.zeros((N_EXPERTS,), jnp.int32).at[flat_e].add(1)
    padded = (counts + MOE_BLOCK - 1) // MOE_BLOCK * MOE_BLOCK
    start = jnp.cumsum(counts) - counts
    pend = jnp.cumsum(padded)
    pstart = pend - padded
    rank = jnp.arange(n_slots, dtype=jnp.int32) - start[sorted_e]
    dest = jnp.zeros((n_slots,), jnp.int32).at[order].set(pstart[sorted_e] + rank)
    cap = -(-(n_slots + N_EXPERTS * MOE_BLOCK) // MOE_BLOCK) * MOE_BLOCK
    src = jnp.full((cap,), n, jnp.int32).at[dest].set(jnp.arange(n_slots, dtype=jnp.int32) // TOP_K)
    t_pad = jnp.concatenate([t, jnp.zeros((1, shp[-1]), t.dtype)], axis=0)
    n_blocks = cap // MOE_BLOCK
    xb = t_pad[src].reshape(n_blocks, MOE_BLOCK, shp[-1])
    blk_start = jnp.arange(n_blocks, dtype=jnp.int32) * MOE_BLOCK
    blk_e = jnp.minimum(jnp.searchsorted(pend, blk_start, side='right'), N_EXPERTS - 1)

    def expert_block(args):
        xe, e = args
        return swiglu(xe, w1[e], w3[e], w2[e])

    yb = lax.map(expert_block, (xb, blk_e)).reshape(cap, shp[-1])
    y = yb[dest].reshape(n, TOP_K, shp[-1])
    return jnp.einsum('nk,nkd->nd', wts, y).reshape(shp)


def setup_inputs(seed: int = 0) -> dict:
    key = jax.random.key(seed)
    ks = jax.random.split(key, 22)
    D = D_MODEL

    def nrm(k, shape, s):
        return jax.random.normal(k, shape, f32) * s

    return {
        'x': nrm(ks[0], (BATCH, SEQ, D), 1.0),
        'c': nrm(ks[1], (BATCH, D), 1.0),
        'ctx': nrm(ks[2], (BATCH, CTX_LEN, D), 1.0),
        'c_ctx': nrm(ks[3], (D,), 1.0),
        'w_mod': nrm(ks[4], (DEPTH, D, 6 * D), 0.5 * D ** -0.5),
        'b_mod': nrm(ks[5], (DEPTH, 6 * D), 0.02),
        'norm1': 1.0 + nrm(ks[6], (DEPTH, D), 0.1),
        'norm2': 1.0 + nrm(ks[7], (DEPTH, D), 0.1),
        'w_in': nrm(ks[8], (DEPTH, D, IN_COLS), D ** -0.5),
        'qk_gain': 1.0 + nrm(ks[9], (DEPTH, 6, HEAD_DIM), 0.1),
        'a_sink': nrm(ks[10], (DEPTH, A_HEADS), 1.0),
        'b_lambda': nrm(ks[11], (DEPTH, 4, HEAD_DIM), 0.1),
        'b_subln': 1.0 + nrm(ks[12], (DEPTH, B_VDIM), 0.1),
        'w_branch': nrm(ks[13], (DEPTH, 3, BRANCH_WIDTH, D), BRANCH_WIDTH ** -0.5),
        'w_out': nrm(ks[14], (DEPTH, D, D), D ** -0.5),
        'dense_w1': nrm(ks[15], (N_DENSE, D, D_FF), D ** -0.5),
        'dense_w3': nrm(ks[16], (N_DENSE, D, D_FF), D ** -0.5),
        'dense_w2': nrm(ks[17], (N_DENSE, D_FF, D), D_FF ** -0.5),
        'moe_router': nrm(ks[18], (N_MOE, D, N_EXPERTS), D ** -0.5),
        'moe_w1': nrm(ks[19], (N_MOE, N_EXPERTS, D, D_FF_EXPERT), D ** -0.5),
        'moe_w3': nrm(ks[20], (N_MOE, N_EXPERTS, D, D_FF_EXPERT), D ** -0.5),
        'moe_w2': nrm(ks[21], (N_MOE, N_EXPERTS, D_FF_EXPERT, D), D_FF_EXPERT ** -0.5),
    }


def channel_mixer(t, l, dense_w1, dense_w3, dense_w2, moe_router, moe_w1, moe_w3, moe_w2):
    if l % 2 == 0:
        i = l // 2
        return swiglu(t, dense_w1[i], dense_w3[i], dense_w2[i])
    i = l // 2
    return moe_swiglu(t, moe_router[i], moe_w1[i], moe_w3[i], moe_w2[i])


def reference(x, c, ctx, c_ctx, w_mod, b_mod, norm1, norm2, w_in, qk_gain, a_sink, b_lambda,
              b_subln, w_branch, w_out, dense_w1, dense_w3, dense_w2, moe_router, moe_w1,
              moe_w3, moe_w2):
    for l in range(DEPTH):
        with_ctx = l < DEPTH - 1
        lam_init = 0.8 - 0.6 * math.exp(-0.3 * l)
        m = (jax.nn.silu(c) @ w_mod[l] + b_mod[l])[:, None, :]
        mc = jax.nn.silu(c_ctx) @ w_mod[l] + b_mod[l]
        sh1, sc1, g1, sh2, sc2, g2 = jnp.split(m, 6, axis=-1)
        csh1, csc1, cg1, csh2, csc2, cg2 = jnp.split(mc, 6, axis=-1)
        h = rmsnorm(x, norm1[l]) * (1.0 + sc1) + sh1
        hc = rmsnorm(ctx, norm1[l]) * (1.0 + csc1) + csh1
        y, yc = mixer(h, hc, w_in[l], qk_gain[l], a_sink[l], b_lambda[l], b_subln[l],
                      w_branch[l], w_out[l], lam_init, with_ctx)
        x = x + g1 * y
        h2 = rmsnorm(x, norm2[l]) * (1.0 + sc2) + sh2
        x = x + g2 * channel_mixer(h2, l, dense_w1, dense_w3, dense_w2, moe_router, moe_w1, moe_w3, moe_w2)
        if with_ctx:
            ctx = ctx + cg1 * yc
            hc2 = rmsnorm(ctx, norm2[l]) * (1.0 + csc2) + csh2
            ctx = ctx + cg2 * channel_mixer(hc2, l, dense_w1, dense_w3, dense_w2, moe_router, moe_w1, moe_w3, moe_w2)
    return x
```

```python
import contextlib
import math
import numpy as np
import concourse.bass as bass
import concourse.mybir as mybir
from concourse.bass_utils import run_bass_kernel_spmd

F32 = mybir.dt.float32
BF16 = mybir.dt.bfloat16
AF = mybir.ActivationFunctionType
ALU = mybir.AluOpType
AX = mybir.AxisListType

ENGS = ["sync", "scalar", "gpsimd", "vector", "tensor"]
SAME_ENGINE_SYNC = {"scalar": True, "vector": True, "gpsimd": True, "tensor": False, "sync": False}
N_DMA_SEMS = 72


class Cfg:
    def __init__(self, D=2048, S=8192, CTX=256, DFF=5632, DFFE=7168, NE=8, GRID_W=64, TB=512, DEPTH=2, mode="fused"):
        self.mode = mode
        self.D, self.S, self.CTX, self.DFF, self.DFFE, self.NE, self.GRID_W, self.TB, self.DEPTH = \
            D, S, CTX, DFF, DFFE, NE, GRID_W, TB, DEPTH
        self.DC = D // 128
        self.NT = S + CTX
        self.OWN = S // 4
        self.HD = 128
        self.IN_COLS = 6144 + 3 * D
        self.EPS = 1e-6
        self.segs = [("aq", 1024, "q", 0, 0), ("ak", 256, "k", 1, 0), ("av", 256, "v", -1, 0),
                     ("bq", 1024, "q", 2, 8), ("bk", 1024, "k", 3, 2), ("bv", 1024, "v", -1, 256),
                     ("cq", 1024, "q", 4, 16), ("ck", 256, "k", 5, 10), ("cv", 256, "v", -1, 1280),
                     ("ga", D, "g", -1, 0), ("gb", D, "g", -1, D), ("gc", D, "g", -1, 2 * D)]


class Buf:
    __slots__ = ("name", "last_write", "readers", "pool_idx", "dma_total")

    def __init__(self, name):
        self.name = name
        self.last_write = None
        self.readers = {}
        self.pool_idx = None
        self.dma_total = 0


class Op:
    __slots__ = ("eng", "fn", "deps", "is_dma", "sig", "sigval", "signal", "idx", "pool_idx")


class Sched:
    def __init__(self, nc, st):
        self.nc = nc
        self.ops = []
        self.flushed = 0
        self.esem = {e: st.enter_context(nc.semaphore("es_" + e)) for e in ENGS}
        self.dsem = [st.enter_context(nc.semaphore("ds_%d" % i)) for i in range(N_DMA_SEMS)]
        self.dtotal = [0] * N_DMA_SEMS
        self.ecount = {e: 0 for e in ENGS}
        self.known = {e: {} for e in ENGS}
        self.stage_bufs = []
        self.pool_next = 0
        self.pool_used = set()

    def buf(self, name):
        b = Buf(name)
        self.stage_bufs.append(b)
        return b

    def _pool(self, b):
        if b.pool_idx is None:
            for _ in range(N_DMA_SEMS):
                i = self.pool_next
                self.pool_next = (self.pool_next + 1) % N_DMA_SEMS
                if i not in self.pool_used:
                    break
            else:
                raise RuntimeError("out of dma sems")
            self.pool_used.add(i)
            b.pool_idx = i
            b.dma_total = self.dtotal[i]
        return b.pool_idx

    def _add(self, eng, fn, reads, writes, is_dma=False, sig=None):
        op = Op()
        op.eng, op.fn, op.is_dma, op.sig = eng, fn, is_dma, sig
        op.sigval, op.signal, op.idx, op.pool_idx = None, False, len(self.ops), None
        deps = set()
        for b in reads:
            if b.last_write is not None:
                deps.add(b.last_write)
        for b in writes:
            if b.last_write is not None:
                lw = self.ops[b.last_write]
                if not (is_dma and lw.is_dma and lw.sig is sig):
                    deps.add(b.last_write)
            for r in b.readers.values():
                deps.add(r)
        op.deps = deps
        self.ops.append(op)
        key = ("dma", op.idx) if is_dma else eng
        for b in reads:
            b.readers[key] = op.idx
        for b in writes:
            b.last_write = op.idx
            b.readers = {}
        if is_dma:
            op.pool_idx = self._pool(sig)
            sig.dma_total += 16
            self.dtotal[op.pool_idx] = sig.dma_total
            op.sigval = sig.dma_total
        return op

    def op(self, eng, fn, reads=(), writes=()):
        return self._add(eng, fn, list(reads), list(writes))

    def dma(self, eng, fn, reads=(), writes=(), sig=None):
        return self._add(eng, fn, list(reads), list(writes), True, sig)

    def _event(self, o):
        if o.is_dma:
            return self.dsem[o.pool_idx], o.sigval
        return self.esem[o.eng], o.sigval

    def flush(self, final=False):
        nc = self.nc
        ops = self.ops
        new = ops[self.flushed:]
        base = self.flushed
        per_eng = {e: [] for e in ENGS}
        for o in new:
            per_eng[o.eng].append(o)
        for o in new:
            for d in o.deps:
                od = ops[d]
                if d >= base and not od.is_dma:
                    if od.eng != o.eng or SAME_ENGINE_SYNC[o.eng] or o.is_dma:
                        od.signal = True
        for e in ENGS:
            for o in reversed(per_eng[e]):
                if not o.is_dma:
                    o.signal = True
                    break
        for o in new:
            if not o.is_dma and o.signal:
                self.ecount[o.eng] += 1
                o.sigval = self.ecount[o.eng]
        finals = []
        for e in ENGS:
            if self.ecount[e] > 0:
                finals.append((self.esem[e], self.ecount[e]))
        for i in range(N_DMA_SEMS):
            if self.dtotal[i] > 0:
                finals.append((self.dsem[i], self.dtotal[i]))

        with nc.Block() as block:
            def make(engname):
                def body(eng):
                    known = self.known[engname]
                    for o in per_eng[engname]:
                        need = {}
                        for d in o.deps:
                            if d < base:
                                continue
                            od = ops[d]
                            if (not od.is_dma) and od.eng == engname and not (SAME_ENGINE_SYNC[engname] or o.is_dma):
                                continue
                            s, v = self._event(od)
                            if need.get(id(s), (None, 0))[1] < v:
                                need[id(s)] = (s, v)
                        for sid, (s, v) in need.items():
                            if known.get(sid, 0) < v:
                                eng.wait_ge(s, v)
                                known[sid] = v
                        ins = o.fn(eng)
                        if o.is_dma:
                            ins.then_inc(self.dsem[o.pool_idx], 16)
                        elif o.signal:
                            ins.then_inc(self.esem[engname], 1)
                    for (s, v) in finals:
                        if known.get(id(s), 0) < v:
                            eng.wait_ge(s, v)
                            known[id(s)] = v
                return body

            for e in ENGS:
                getattr(block, e)(make(e))
        self.flushed = len(ops)
        self.stage_bufs = []
        self.pool_used = set()


class TileT:
    __slots__ = ("t", "b")

    def __init__(self, t, b):
        self.t, self.b = t, b


class Builder:
    def __init__(self, cfg):
        self.cfg = cfg

    def sb(self, st, name, shape, dt):
        self.uid = getattr(self, "uid", 0) + 1
        name = "%s_u%d" % (name, self.uid)
        t = st.enter_context(self.nc.sbuf_tensor(name, list(shape), dt))
        return TileT(t, self.S.buf(name))

    def rebuf(self, tiles):
        for t in tiles:
            t.b = self.S.buf(t.b.name)

    def build(self):
        cfg = self.cfg
        D, NT, DC, OWN = cfg.D, cfg.NT, cfg.DC, cfg.OWN
        nc = bass.Bass("TRN2", target_bir_lowering=False)
        self.nc = nc
        L = cfg.DEPTH if cfg.mode == "fused" else 1

        def din(name, shape, dt=F32):
            return nc.dram_tensor(name, list(shape), dt, kind="ExternalInput").ap()

        self.xT = din("xT", [D, NT])
        self.cosT = din("cosT", [128, NT])
        self.sinT = din("sinT", [128, NT])
        self.posrow = din("posrow", [1, NT])
        self.poscol = din("poscol", [128, NT // 128])
        self.consts = din("consts", [128, 384])
        self.cvec = din("cvec", [2, D])
        def tsh(K, N):
            return [(N + 511) // 512, 128, K // 128, 512]

        self.w_mod = din("w_mod", [L] + tsh(D, 6 * D))
        self.b_mod = din("b_mod", [L, 6 * D])
        self.norm1 = din("norm1", [L, D])
        self.norm2 = din("norm2", [L, D])
        self.w_in = din("w_in", [L] + tsh(D, cfg.IN_COLS))
        self.qk_gain = din("qk_gain", [L, 6 * 128])
        self.a_sink = din("a_sink", [L, 8])
        self.b_lambda = din("b_lambda", [L, 4 * 128])
        self.b_subln = din("b_subln", [L, 256])
        self.w_branch = din("w_branch", [L, 3] + tsh(1024, D))
        self.w_out = din("w_out", [L] + tsh(D, D))
        if cfg.mode == "L1":
            self.dense_w1 = din("dense_w1", [1] + tsh(128, 512))
            self.dense_w3 = din("dense_w3", [1] + tsh(128, 512))
            self.dense_w2 = din("dense_w2", [1] + tsh(128, 512))
        else:
            self.dense_w1 = din("dense_w1", [1] + tsh(D, cfg.DFF))
            self.dense_w3 = din("dense_w3", [1] + tsh(D, cfg.DFF))
            self.dense_w2 = din("dense_w2", [1] + tsh(cfg.DFF, D))
        self.moe_router = din("moe_router", [1, D, cfg.NE])
        if cfg.mode == "L0":
            self.moe_w1 = din("moe_w1", [cfg.NE] + tsh(128, 512))
            self.moe_w3 = din("moe_w3", [cfg.NE] + tsh(128, 512))
            self.moe_w2 = din("moe_w2", [cfg.NE] + tsh(128, 512))
        else:
            self.moe_w1 = din("moe_w1", [cfg.NE] + tsh(D, cfg.DFFE))
            self.moe_w3 = din("moe_w3", [cfg.NE] + tsh(D, cfg.DFFE))
            self.moe_w2 = din("moe_w2", [cfg.NE] + tsh(cfg.DFFE, D))
        self.yT = nc.dram_tensor("yT", [D, OWN + (cfg.CTX if cfg.mode == "L0" else 0)], F32, kind="ExternalOutput").ap()
        dbg = getattr(self, "debug", False)
        kw = {"kind": "ExternalOutput"} if dbg else {}
        self.QT = nc.dram_tensor("QT", [24, 128, NT], BF16, **kw).ap()
        self.KT = nc.dram_tensor("KT", [12, 128, NT], BF16, **kw).ap()
        self.VVh = nc.dram_tensor("VVh", [8, 128, (NT // 128) * 256], BF16, **kw).ap()
        self.GT = nc.dram_tensor("GT", [3 * D, NT], BF16, **kw).ap()
        self.X1T = nc.dram_tensor("X1T", [D, NT], F32, **kw).ap()
        self.X2T = nc.dram_tensor("X2T", [D, NT], F32, **kw).ap()

        with contextlib.ExitStack() as st0:
            self.S = Sched(nc, st0)
            S = self.S
            self.ps = [TileT(st0.enter_context(nc.psum_tensor("ps%d" % i, [128, 512], F32)), S.buf("ps%d" % i))
                       for i in range(8)]
            P = {}
            self.P = P
            for name, shape, dt in [("ident_f", [128, 128], F32), ("ones_f", [128, 128], F32),
                                    ("ones_b", [128, 128], BF16), ("prot_b", [128, 128], BF16),
                                    ("modA1", [128, 2, DC], F32), ("modB1", [128, 2, DC], F32), ("modG1", [128, 2, DC], F32),
                                    ("modA2", [128, 2, DC], F32), ("modB2", [128, 2, DC], F32), ("modG2", [128, 2, DC], F32),
                                    ("gainT", [128, 6], F32), ("negb", [128, 3], F32), ("esink", [128, 8], F32),
                                    ("neglam", [128, 1], F32), ("sgT", [128, 2], F32), ("epsc", [128, 1], F32)]:
                P[name] = self.sb(st0, name, shape, dt)
            self.persist = list(P.values())
            self.stage_consts()
            xin = self.xT
            if cfg.mode == "fused":
                plan = [(l, l, l == L - 1, OWN if l == L - 1 else getattr(cfg, 'L0_OWN', cfg.S)) for l in range(L)]
            elif cfg.mode == "L0":
                plan = [(0, 0, False, OWN)]
            else:
                plan = [(0, 1, True, OWN)]
            for (l, lsem, last, own_lat) in plan:
                self.lsem = lsem
                blocks_all = [(c0, cfg.TB, False) for c0 in range(0, cfg.S, cfg.TB)]
                cw = min(cfg.TB, cfg.CTX)
                ctx_blocks = [(cfg.S + c0, cw, True) for c0 in range(0, cfg.CTX, cw)]
                own_blocks = [b for b in blocks_all if b[0] < own_lat] + ([] if last else ctx_blocks)
                self.stage_mod(l)
                self.stage_proj(l, xin, blocks_all + ctx_blocks, own_lat, with_ctx=not last)
                self.stage_attn(l, xin, own_blocks)
                if cfg.mode == "L0":
                    xout, xmap = self.yT, (lambda c0: c0 if c0 < cfg.S else OWN + (c0 - cfg.S))
                else:
                    xout, xmap = (self.yT if last else self.X2T), (lambda c0: c0)
                self.xmap = xmap
                self.stage_ffn(l, own_blocks, xout)
                xin = self.X2T
            S.flush(final=True)
        return nc

    def psb(self, i):
        return self.ps[i]

    def begin_stage(self):
        self.rebuf(self.persist)
        self.rebuf(self.ps)

    def row_to_cols(self, st, row_ap, n, name):
        S, nc = self.S, self.nc
        rowt = self.sb(st, name + "_row", [1, n * 128], F32)
        out = self.sb(st, name + "_cols", [128, n], F32)
        S.dma("sync", lambda e: e.dma_start(out=rowt.t[:], in_=row_ap), writes=[rowt.b], sig=rowt.b)
        ps = self.psb(7)
        one = self.P["ones_f"]
        for j in range(n):
            S.op("tensor", lambda e, j=j: e.matmul(ps.t[:, j:j + 1], lhsT=rowt.t[0:1, j * 128:(j + 1) * 128],
                                                   rhs=one.t[0:1, 0:1], start=True, stop=True),
                 reads=[rowt.b, one.b], writes=[ps.b])
        S.op("vector", lambda e: e.tensor_copy(out=out.t[:], in_=ps.t[:, 0:n]), reads=[ps.b], writes=[out.b])
        return out

    def stage_consts(self):
        S, nc, P = self.S, self.nc, self.P
        with contextlib.ExitStack() as st:
            cst = self.sb(st, "cst", [128, 384], F32)
            S.dma("sync", lambda e: e.dma_start(out=cst.t[:], in_=self.consts), writes=[cst.b], sig=cst.b)
            S.op("vector", lambda e: e.tensor_copy(out=P["ident_f"].t[:], in_=cst.t[:, 0:128]), reads=[cst.b], writes=[P["ident_f"].b])
            S.op("vector", lambda e: e.tensor_copy(out=P["prot_b"].t[:], in_=cst.t[:, 128:256]), reads=[cst.b], writes=[P["prot_b"].b])
            S.op("vector", lambda e: e.tensor_copy(out=P["ones_f"].t[:], in_=cst.t[:, 256:384]), reads=[cst.b], writes=[P["ones_f"].b])
            S.op("vector", lambda e: e.tensor_copy(out=P["ones_b"].t[:], in_=cst.t[:, 256:384]), reads=[cst.b], writes=[P["ones_b"].b])
            S.op("vector", lambda e: e.memset(P["epsc"].t[:], self.cfg.EPS), writes=[P["epsc"].b])
            S.flush()

    def stage_mod(self, l):
        cfg, S, nc, P = self.cfg, self.S, self.nc, self.P
        D, DC = cfg.D, cfg.DC
        lam_init = 0.8 - 0.6 * math.exp(-0.3 * self.lsem)
        self.begin_stage()
        with contextlib.ExitStack() as st:
            sc = self.sb(st, "silc", [128, DC, 2], BF16)
            for v in range(2):
                cc = self.row_to_cols(st, self.cvec[v:v + 1, :], DC, "c%d" % v)
                S.op("scalar", lambda e, cc=cc, v=v: e.activation(out=sc.t[:, :, v], in_=cc.t[:], func=AF.Silu),
                     reads=[cc.b], writes=[sc.b])
            n1 = self.row_to_cols(st, self.norm1[l:l + 1, :], DC, "n1")
            n2 = self.row_to_cols(st, self.norm2[l:l + 1, :], DC, "n2")
            brow = self.sb(st, "brow", [1, 6 * D], BF16)
            for c0 in range(0, 6 * D, 2048):
                c1 = min(6 * D, c0 + 2048)
                S.dma("gpsimd", lambda e, c0=c0, c1=c1: e.dma_start(out=brow.t[0:1, c0:c1], in_=self.b_mod[l:l + 1, c0:c1]),
                      writes=[brow.b], sig=brow.b)
            modT = self.sb(st, "modT", [128, 6 * DC, 2], F32)
            wsl = [self.sb(st, "wm%d" % i, [128, DC, 512], BF16) for i in range(2)]
            psm = self.psb(0)
            npan = 6 * D // 512
            for pn in range(npan):
                w = wsl[pn % 2]
                self.load_panel(w, self.w_mod[l], 0, D, pn * 512, 512)
                for j in range(4):
                    ch = pn * 4 + j
                    for kc in range(DC):
                        S.op("tensor", lambda e, w=w, kc=kc, j=j, ch=ch: e.matmul(
                            psm.t[:, ch * 2:ch * 2 + 2], lhsT=w.t[:, kc, j * 128:(j + 1) * 128], rhs=sc.t[:, kc, :],
                            start=(kc == 0), stop=False), reads=[w.b, sc.b], writes=[psm.b])
                    S.op("tensor", lambda e, ch=ch: e.matmul(
                        psm.t[:, ch * 2:ch * 2 + 2], lhsT=brow.t[0:1, ch * 128:(ch + 1) * 128], rhs=P["ones_b"].t[0:1, 0:2],
                        start=False, stop=True), reads=[brow.b, P["ones_b"].b], writes=[psm.b])
            S.op("vector", lambda e: e.tensor_copy(out=modT.t[:].rearrange("p a b -> p (a b)"), in_=psm.t[:, 0:12 * DC]),
                 reads=[psm.b], writes=[modT.b])
            for v in range(2):
                for (nm, nrm, i_sh, i_sc, i_g, A, B, G) in [(1, n1, 0, 1, 2, "modA1", "modB1", "modG1"),
                                                            (2, n2, 3, 4, 5, "modA2", "modB2", "modG2")]:
                    S.op("vector", lambda e, v=v, nrm=nrm, i_sc=i_sc, A=A: e.scalar_tensor_tensor(
                        out=P[A].t[:, v, :], in0=modT.t[:, i_sc * DC:(i_sc + 1) * DC, v], scalar=1.0, in1=nrm.t[:],
                        op0=ALU.add, op1=ALU.mult), reads=[modT.b, nrm.b], writes=[P[A].b])
                    S.op("vector", lambda e, v=v, i_sh=i_sh, B=B: e.tensor_copy(
                        out=P[B].t[:, v, :], in_=modT.t[:, i_sh * DC:(i_sh + 1) * DC, v]), reads=[modT.b], writes=[P[B].b])
                    S.op("vector", lambda e, v=v, i_g=i_g, G=G: e.tensor_copy(
                        out=P[G].t[:, v, :], in_=modT.t[:, i_g * DC:(i_g + 1) * DC, v]), reads=[modT.b], writes=[P[G].b])
            gb = self.sb(st, "gb", [128, 6, 128], F32)
            S.dma("sync", lambda e: e.dma_start(out=gb.t[:].rearrange("p a b -> p (a b)"),
                                                in_=self.qk_gain[l:l + 1, :].partition_broadcast(128).rearrange("p a b -> p (a b)")),
                  writes=[gb.b], sig=gb.b)
            gm = self.sb(st, "gm", [128, 6], F32)
            S.op("vector", lambda e: e.tensor_reduce(out=gm.t[:], in_=gb.t[:], axis=AX.X, op=ALU.max, apply_absolute_value=True),
                 reads=[gb.b], writes=[gm.b])
            for br in range(3):
                S.op("vector", lambda e, br=br: e.scalar_tensor_tensor(
                    out=P["negb"].t[:, br:br + 1], in0=gm.t[:, 2 * br:2 * br + 1], scalar=-math.sqrt(128.0),
                    in1=gm.t[:, 2 * br + 1:2 * br + 2], op0=ALU.mult, op1=ALU.mult), reads=[gm.b], writes=[P["negb"].b])
            with nc.allow_non_contiguous_dma(reason="tiny transposed param load"):
                S.dma("sync", lambda e: e.dma_start(out=P["gainT"].t[:], in_=self.qk_gain[l, :].rearrange("(g p) -> p g", p=128),
                                                    allow_slow_non_contiguous=True),
                      writes=[P["gainT"].b], sig=P["gainT"].b)
                S.dma("sync", lambda e: e.dma_start(out=P["sgT"].t[:], in_=self.b_subln[l, :].rearrange("(g p) -> p g", p=128),
                                                    allow_slow_non_contiguous=True),
                      writes=[P["sgT"].b], sig=P["sgT"].b)
            S.op("vector", lambda e: e.tensor_scalar(out=P["sgT"].t[:], in0=P["sgT"].t[:], scalar1=(1.0 - lam_init), scalar2=None,
                                                     op0=ALU.mult), reads=[P["sgT"].b], writes=[P["sgT"].b])
            sk = self.sb(st, "sk", [128, 8], F32)
            S.dma("sync", lambda e: e.dma_start(out=sk.t[:], in_=self.a_sink[l:l + 1, :].partition_broadcast(128).rearrange("p a b -> p (a b)")),
                  writes=[sk.b], sig=sk.b)
            S.op("scalar", lambda e: e.activation(out=P["esink"].t[:], in_=sk.t[:], func=AF.Exp, bias=P["negb"].t[:, 0:1], scale=1.0),
                 reads=[sk.b, P["negb"].b], writes=[P["esink"].b])
            lb = self.sb(st, "lb", [128, 4, 128], F32)
            S.dma("sync", lambda e: e.dma_start(out=lb.t[:].rearrange("p a b -> p (a b)"),
                                                in_=self.b_lambda[l:l + 1, :].partition_broadcast(128).rearrange("p a b -> p (a b)")),
                  writes=[lb.b], sig=lb.b)
            pr = self.sb(st, "lpr", [128, 2, 128], F32)
            ss = self.sb(st, "lss", [128, 2], F32)
            ee = self.sb(st, "lee", [128, 2], F32)
            for i in range(2):
                S.op("vector", lambda e, i=i: e.tensor_tensor(out=pr.t[:, i, :], in0=lb.t[:, 2 * i, :], in1=lb.t[:, 2 * i + 1, :], op=ALU.mult),
                     reads=[lb.b], writes=[pr.b])
            S.op("vector", lambda e: e.tensor_reduce(out=ss.t[:], in_=pr.t[:], axis=AX.X, op=ALU.add), reads=[pr.b], writes=[ss.b])
            S.op("scalar", lambda e: e.activation(out=ee.t[:], in_=ss.t[:], func=AF.Exp), reads=[ss.b], writes=[ee.b])
            S.op("vector", lambda e: e.scalar_tensor_tensor(out=P["neglam"].t[:], in0=ee.t[:, 1:2], scalar=-lam_init, in1=ee.t[:, 0:1],
                                                            op0=ALU.add, op1=ALU.subtract), reads=[ee.b], writes=[P["neglam"].b])
            S.flush()

    def load_panel(self, w, wsrc, r0, nrows, c0, ncols, eng="gpsimd", k_off=0):
        S = self.S
        nk = nrows // 128
        k0 = r0 // 128
        pn, coff = c0 // 512, c0 % 512
        assert coff + ncols <= 512
        src = wsrc[pn]
        step = 4
        for a0 in range(0, nk, step):
            a1 = min(nk, a0 + step)
            S.dma(eng, lambda e, a0=a0, a1=a1: e.dma_start(out=w.t[:, k_off + a0:k_off + a1, 0:ncols],
                                                           in_=src[:, k0 + a0:k0 + a1, coff:coff + ncols], max_dma_last_dim=8192),
                  writes=[w.b], sig=w.b)

    def modnorm(self, st_tiles, xblk, w, A, B, v, hT, hook=None):
        cfg, S, P = self.cfg, self.S, self.P
        DC, D = cfg.DC, cfg.D
        sq, rr, rinv, tmp2 = st_tiles["sq"], st_tiles["rr"], st_tiles["rinv"], st_tiles["tmpn"]
        pss = self.psb(7)
        S.op("scalar", lambda e: e.activation(out=sq.t[:, 0:DC, 0:w], in_=xblk.t[:, 0:DC, 0:w], func=AF.Square),
             reads=[xblk.b], writes=[sq.b])
        for dc in range(DC):
            S.op("tensor", lambda e, dc=dc: e.matmul(pss.t[:, 0:w], lhsT=P["ones_b"].t[:], rhs=sq.t[:, dc, 0:w],
                                                     start=(dc == 0), stop=(dc == DC - 1)),
                 reads=[P["ones_b"].b, sq.b], writes=[pss.b])
        S.op("scalar", lambda e: e.activation(out=rr.t[:, 0:w], in_=pss.t[:, 0:w], func=AF.Sqrt, bias=P["epsc"].t[:, 0:1], scale=1.0 / D),
             reads=[pss.b, P["epsc"].b], writes=[rr.b])
        S.op("vector", lambda e: e.reciprocal(out=rinv.t[:, 0:w], in_=rr.t[:, 0:w]), reads=[rr.b], writes=[rinv.b])
        for dc in range(DC):
            t2 = tmp2[dc % 2]
            S.op("vector", lambda e, dc=dc, t2=t2: e.scalar_tensor_tensor(
                out=t2.t[:, 0:w], in0=xblk.t[:, dc, 0:w], scalar=P[A].t[:, v, dc:dc + 1], in1=rinv.t[:, 0:w],
                op0=ALU.mult, op1=ALU.mult), reads=[xblk.b, P[A].b, rinv.b], writes=[t2.b])
            if hook is None:
                S.op("scalar", lambda e, dc=dc, t2=t2: e.activation(out=hT.t[:, dc, 0:w], in_=t2.t[:, 0:w], func=AF.Identity,
                                                                    bias=P[B].t[:, v, dc:dc + 1], scale=1.0),
                     reads=[t2.b, P[B].b], writes=[hT.b])
            else:
                S.op("scalar", lambda e, dc=dc, t2=t2: e.activation(out=t2.t[:, 0:w], in_=t2.t[:, 0:w], func=AF.Identity,
                                                                    bias=P[B].t[:, v, dc:dc + 1], scale=1.0),
                     reads=[t2.b, P[B].b], writes=[t2.b])
                S.op("vector", lambda e, dc=dc, t2=t2: e.tensor_copy(out=hT.t[:, dc, 0:w], in_=t2.t[:, 0:w]),
                     reads=[t2.b], writes=[hT.b])
                hook(dc, t2)

    def norm_tiles(self, st, sq=None):
        cfg = self.cfg
        return {"sq": sq if sq is not None else self.sb(st, "sq", [128, cfg.DC, cfg.TB], BF16),
                "rr": self.sb(st, "rr", [128, cfg.TB], F32),
                "rinv": self.sb(st, "rinv", [128, cfg.TB], F32),
                "tmpn": [self.sb(st, "tmpn%d" % i, [128, cfg.TB], F32) for i in range(2)]}

    def load_xblk(self, xblk, src, c0, w):
        S = self.S
        DC = self.cfg.DC
        v = src.rearrange("(dc p) t -> p dc t", p=128)
        step = 4
        for d0 in range(0, DC, step):
            d1 = min(DC, d0 + step)
            S.dma("sync", lambda e, d0=d0, d1=d1: e.dma_start(out=xblk.t[:, d0:d1, 0:w], in_=v[:, d0:d1, c0:c0 + w]),
                  writes=[xblk.b], sig=xblk.b)

    def stage_proj(self, l, xin, blocks, own_lat, with_ctx):
        cfg, S, nc, P = self.cfg, self.S, self.nc, self.P
        D, DC, TB = cfg.D, cfg.DC, cfg.TB
        self.begin_stage()
        panels = []
        col = 0
        for (nm, ncols, typ, gi, base) in cfg.segs:
            for p0 in range(0, ncols, 512):
                pw = min(512, ncols - p0)
                panels.append((col + p0, pw, typ, gi, base, p0))
            col += ncols
        with contextlib.ExitStack() as st:
            nt = self.norm_tiles(st)
            xblk = self.sb(st, "xblk", [128, DC, TB], F32)
            hT = self.sb(st, "hT", [128, DC, TB], BF16)
            wsl = [self.sb(st, "wp%d" % i, [128, DC, 512], BF16) for i in range(3)]
            cs = [self.sb(st, "cos%d" % i, [128, TB], F32) for i in range(2)]
            sn = [self.sb(st, "sin%d" % i, [128, TB], F32) for i in range(2)]
            sqh = [self.sb(st, "sqh%d" % i, [128, TB], BF16) for i in range(3)]
            rrh = [self.sb(st, "rrh%d" % i, [128, TB], F32) for i in range(3)]
            rih = [self.sb(st, "rih%d" % i, [128, TB], F32) for i in range(3)]
            qn = [self.sb(st, "qn%d" % i, [128, TB], BF16) for i in range(3)]
            t1 = [self.sb(st, "t1%d" % i, [128, TB], F32) for i in range(3)]
            t2 = [self.sb(st, "t2%d" % i, [128, TB], F32) for i in range(3)]
            qo = [self.sb(st, "qo%d" % i, [128, TB], BF16) for i in range(3)]
            vo = [self.sb(st, "vo%d" % i, [128, 512], BF16) for i in range(3)]
            go = [self.sb(st, "go%d" % i, [128, TB], BF16) for i in range(3)]
            cnt = {"w": 0, "q": 0, "v": 0, "g": 0, "ps": 0}
            qB, qC = [], []

            def step():
                if qC:
                    qC.pop(0)()
                if qB:
                    fb, fc = qB.pop(0)
                    fb()
                    qC.append(fc)

            for bi, (c0, w, is_ctx) in enumerate(blocks):
                own = is_ctx and with_ctx or ((not is_ctx) and c0 < own_lat)
                v = 1 if is_ctx else 0
                self.load_xblk(xblk, xin, c0, w)
                c_t, s_t = cs[bi % 2], sn[bi % 2]
                S.dma("sync", lambda e, c_t=c_t, c0=c0, w=w: e.dma_start(out=c_t.t[:, 0:w], in_=self.cosT[:, c0:c0 + w]), writes=[c_t.b], sig=c_t.b)
                S.dma("sync", lambda e, s_t=s_t, c0=c0, w=w: e.dma_start(out=s_t.t[:, 0:w], in_=self.sinT[:, c0:c0 + w]), writes=[s_t.b], sig=s_t.b)
                self.modnorm(nt, xblk, w, "modA1", "modB1", v, hT)
                for (pc0, pw, typ, gi, base, p0) in panels:
                    if not own and typ not in ("k", "v"):
                        continue
                    wp = wsl[cnt["w"] % 3]
                    cnt["w"] += 1
                    self.load_panel(wp, self.w_in[l], 0, D, pc0, pw)
                    if typ == "v":
                        for sub in range(w // 128):
                            ps = self.psb(cnt["ps"] % 4)
                            cnt["ps"] += 1
                            for kc in range(DC):
                                S.op("tensor", lambda e, ps=ps, wp=wp, kc=kc, sub=sub, pw=pw: e.matmul(
                                    ps.t[:, 0:pw], lhsT=hT.t[:, kc, sub * 128:(sub + 1) * 128], rhs=wp.t[:, kc, 0:pw],
                                    start=(kc == 0), stop=(kc == DC - 1)), reads=[hT.b, wp.b], writes=[ps.b])
                            o = vo[cnt["v"] % 3]
                            cnt["v"] += 1
                            S.op("scalar", lambda e, o=o, ps=ps, pw=pw: e.activation(out=o.t[:, 0:pw], in_=ps.t[:, 0:pw], func=AF.Copy),
                                 reads=[ps.b], writes=[o.b])
                            kb = (c0 + sub * 128) // 128
                            vb = base + p0
                            if vb < 256:
                                hl = [(vb // 128 + i, 128, i * 128) for i in range(pw // 128)]
                            elif vb < 1280:
                                hl = [(2 + (vb - 256) // 256 + i, 256, i * 256) for i in range(pw // 256)]
                            else:
                                hl = [(6 + (vb - 1280) // 128 + i, 128, i * 128) for i in range(pw // 128)]
                            for (hh, dv, off) in hl:
                                S.dma("sync", lambda e, o=o, hh=hh, dv=dv, off=off, kb=kb: e.dma_start(
                                    out=self.VVh[hh, :, kb * dv:(kb + 1) * dv], in_=o.t[:, off:off + dv]), reads=[o.b], sig=o.b)
                            step()
                        continue
                    for j in range(pw // 128):
                        ps = self.psb(cnt["ps"] % 4)
                        cnt["ps"] += 1
                        for kc in range(DC):
                            S.op("tensor", lambda e, ps=ps, wp=wp, kc=kc, j=j, w=w: e.matmul(
                                ps.t[:, 0:w], lhsT=wp.t[:, kc, j * 128:(j + 1) * 128], rhs=hT.t[:, kc, 0:w],
                                start=(kc == 0), stop=(kc == DC - 1)), reads=[hT.b, wp.b], writes=[ps.b])
                        if typ == "g":
                            o = go[cnt["g"] % 3]
                            cnt["g"] += 1
                            S.op("scalar", lambda e, o=o, ps=ps, w=w: e.activation(out=o.t[:, 0:w], in_=ps.t[:, 0:w], func=AF.Sigmoid),
                                 reads=[ps.b], writes=[o.b])
                            r0 = base + p0 + j * 128
                            S.dma("sync", lambda e, o=o, r0=r0, c0=c0, w=w: e.dma_start(
                                out=self.GT[r0:r0 + 128, c0:c0 + w], in_=o.t[:, 0:w]), reads=[o.b], sig=o.b)
                            step()
                            continue
                        i2 = cnt["q"] % 3
                        cnt["q"] += 1
                        hidx = base + (p0 // 128) + j
                        ps2 = self.psb(4 + (cnt["q"] % 2))
                        ps3 = self.psb(6 + (cnt["q"] % 2))
                        S.op("scalar", lambda e, i2=i2, ps=ps, w=w: e.activation(out=sqh[i2].t[:, 0:w], in_=ps.t[:, 0:w], func=AF.Square),
                             reads=[ps.b], writes=[sqh[i2].b])

                        def phaseB(i2=i2, ps=ps, ps2=ps2, gi=gi, w=w):
                            S.op("tensor", lambda e: e.matmul(ps2.t[:, 0:w], lhsT=P["ones_b"].t[:], rhs=sqh[i2].t[:, 0:w], start=True, stop=True),
                                 reads=[P["ones_b"].b, sqh[i2].b], writes=[ps2.b])
                            S.op("scalar", lambda e: e.activation(out=rrh[i2].t[:, 0:w], in_=ps2.t[:, 0:w], func=AF.Sqrt,
                                                                  bias=P["epsc"].t[:, 0:1], scale=1.0 / 128),
                                 reads=[ps2.b, P["epsc"].b], writes=[rrh[i2].b])
                            S.op("vector", lambda e: e.reciprocal(out=rih[i2].t[:, 0:w], in_=rrh[i2].t[:, 0:w]),
                                 reads=[rrh[i2].b], writes=[rih[i2].b])
                            S.op("vector", lambda e: e.scalar_tensor_tensor(
                                out=qn[i2].t[:, 0:w], in0=ps.t[:, 0:w], scalar=P["gainT"].t[:, gi:gi + 1], in1=rih[i2].t[:, 0:w],
                                op0=ALU.mult, op1=ALU.mult), reads=[ps.b, P["gainT"].b, rih[i2].b], writes=[qn[i2].b])

                        def phaseC(i2=i2, ps3=ps3, w=w, c_t=c_t, s_t=s_t, typ=typ, hidx=hidx, c0=c0):
                            S.op("tensor", lambda e: e.matmul(ps3.t[:, 0:w], lhsT=P["prot_b"].t[:], rhs=qn[i2].t[:, 0:w], start=True, stop=True),
                                 reads=[P["prot_b"].b, qn[i2].b], writes=[ps3.b])
                            S.op("vector", lambda e: e.tensor_tensor(out=t1[i2].t[:, 0:w], in0=qn[i2].t[:, 0:w], in1=c_t.t[:, 0:w], op=ALU.mult),
                                 reads=[qn[i2].b, c_t.b], writes=[t1[i2].b])
                            S.op("vector", lambda e: e.tensor_tensor(out=t2[i2].t[:, 0:w], in0=ps3.t[:, 0:w], in1=s_t.t[:, 0:w], op=ALU.mult),
                                 reads=[ps3.b, s_t.b], writes=[t2[i2].b])
                            o = qo[cnt["g"] % 3]
                            cnt["g"] += 1
                            S.op("vector", lambda e: e.tensor_tensor(out=o.t[:, 0:w], in0=t1[i2].t[:, 0:w], in1=t2[i2].t[:, 0:w], op=ALU.add),
                                 reads=[t1[i2].b, t2[i2].b], writes=[o.b])
                            dst = self.QT if typ == "q" else self.KT
                            S.dma("sync", lambda e: e.dma_start(out=dst[hidx, :, c0:c0 + w], in_=o.t[:, 0:w]), reads=[o.b], sig=o.b)

                        step()
                        qB.append((phaseB, phaseC))
                while qB or qC:
                    step()
            S.flush()

    def attn_head(self, A, qt, w, kt, kcols, vb, vblks, dvc, negb_ap, negb_buf, masks, extra, out_fn, pset):
        S, P = self.S, self.P
        psS = [self.psb(0), self.psb(1)]
        psO = [self.psb(2 + 3 * pset), self.psb(3 + 3 * pset)]
        psZ = self.psb(4 + 3 * pset)
        E = A["E"]
        nkb = len(kcols)
        scale = 1.0 / math.sqrt(128.0)
        HKB = (self.cfg.NT // 128) // 2

        def emit_s(i):
            kc0 = kcols[i]
            kbuf = kt[1][0 if kc0 < HKB * 128 else 1]
            S.op("tensor", lambda e, i=i, kc0=kc0: e.matmul(psS[i % 2].t[:, 0:w], lhsT=kt[0][:, kc0:kc0 + 128], rhs=qt.t[:, 0:w],
                                                            start=True, stop=True),
                 reads=[kbuf, qt.b], writes=[psS[i % 2].b])

        emit_s(0)
        for i in range(nkb):
            if i + 1 < nkb:
                emit_s(i + 1)
            Ei = E[A["ecnt"] % len(E)]
            A["ecnt"] += 1
            S.op("scalar", lambda e, i=i, Ei=Ei: e.activation(out=Ei.t[:, 0:w], in_=psS[i % 2].t[:, 0:w], func=AF.Exp,
                                                               bias=negb_ap, scale=scale),
                 reads=[psS[i % 2].b, negb_buf], writes=[Ei.b])
            if masks is not None and masks[i] is not None:
                m = masks[i]
                S.op("vector", lambda e, Ei=Ei, m=m: e.tensor_tensor(out=Ei.t[:, 0:w], in0=Ei.t[:, 0:w], in1=m.t[:, 0:w], op=ALU.mult),
                     reads=[Ei.b, m.b], writes=[Ei.b])
            a_t = A["acc"][i % 2]
            a_eng = "vector" if i % 2 == 0 else "gpsimd"
            if i < 2:
                S.op(a_eng, lambda e, Ei=Ei, a_t=a_t: e.tensor_copy(out=a_t.t[:, 0:w], in_=Ei.t[:, 0:w]), reads=[Ei.b], writes=[a_t.b])
            else:
                S.op(a_eng, lambda e, Ei=Ei, a_t=a_t: e.tensor_tensor(out=a_t.t[:, 0:w], in0=a_t.t[:, 0:w], in1=Ei.t[:, 0:w], op=ALU.add),
                     reads=[Ei.b, a_t.b], writes=[a_t.b])
            for c in range(dvc):
                S.op("tensor", lambda e, i=i, Ei=Ei, c=c: e.matmul(psO[c].t[:, 0:w], lhsT=vb[0](vblks[i], c),
                                                                   rhs=Ei.t[:, 0:w], start=(i == 0), stop=(i == nkb - 1)),
                     reads=[vb[1][0 if vblks[i] < HKB else 1], Ei.b], writes=[psO[c].b])
        a0, a1 = A["acc"]
        if nkb >= 2:
            S.op("vector", lambda e: e.tensor_tensor(out=a0.t[:, 0:w], in0=a0.t[:, 0:w], in1=a1.t[:, 0:w], op=ALU.add),
                 reads=[a0.b, a1.b], writes=[a0.b])
        S.op("tensor", lambda e: e.matmul(psZ.t[:, 0:w], lhsT=P["ones_f"].t[:], rhs=a0.t[:, 0:w], start=True, stop=True),
             reads=[P["ones_f"].b, a0.b], writes=[psZ.b])
        rz = A["rz"][A["rcnt"] % 2]
        A["rcnt"] += 1
        if extra is not None:
            S.op("vector", lambda e: e.tensor_scalar(out=rz.t[:, 0:w], in0=psZ.t[:, 0:w], scalar1=extra[0], scalar2=None, op0=ALU.add),
                 reads=[psZ.b, extra[1]], writes=[rz.b])
            S.op("vector", lambda e: e.reciprocal(out=rz.t[:, 0:w], in_=rz.t[:, 0:w]), reads=[rz.b], writes=[rz.b])
        else:
            S.op("vector", lambda e: e.reciprocal(out=rz.t[:, 0:w], in_=psZ.t[:, 0:w]), reads=[psZ.b], writes=[rz.b])
        for c in range(dvc):
            out_fn(c, psO[c], rz)

    def stage_attn(self, l, xin, own_blocks):
        cfg, S, nc, P = self.cfg, self.S, self.nc, self.P
        D, DC, TB, NT, Sq = cfg.D, cfg.DC, cfg.TB, cfg.NT, cfg.S
        NKB = NT // 128
        self.begin_stage()
        with contextlib.ExitStack() as st:
            A = {"E": [self.sb(st, "E%d" % i, [128, TB], BF16) for i in range(4)], "ecnt": 0,
                 "rz": [self.sb(st, "rz%d" % i, [128, TB], F32) for i in range(2)], "rcnt": 0,
                 "acc": [self.sb(st, "acc%d" % i, [128, TB], F32) for i in range(2)]}
            ktb = [self.sb(st, "kt%d" % i, [128, NT], BF16) for i in range(1)]
            vbuf = self.sb(st, "vbuf", [128, NKB, 256], BF16)
            HKB = NKB // 2
            kt_hb = [ktb[0].b, S.buf("kt_h1")]
            v_hb = [vbuf.b, S.buf("v_h1")]
            qtl = [self.sb(st, "qt%d" % i, [128, TB], BF16) for i in range(3)]
            oT = [self.sb(st, "oT%d" % i, [128, 8, TB], BF16) for i in range(3)]
            yT = self.sb(st, "yT", [128, DC, TB], BF16)
            wsl = [self.sb(st, "wq%d" % i, [128, 16, 512], BF16) for i in range(2)]
            posq = self.sb(st, "posq", [128, TB], F32)
            posk = self.sb(st, "posk", [128, NKB], F32)
            nband = TB // 128 + 2
            mk = [self.sb(st, "mk%d" % i, [128, TB], BF16) for i in range(nband)]
            dtmp = self.sb(st, "dtmp", [128, TB], F32)
            o12 = [self.sb(st, "o12_%d" % i, [128, 2, TB], F32) for i in range(2)]
            obd = self.sb(st, "obd", [128, 2, TB], F32)
            obq = self.sb(st, "obq", [128, 2, TB], BF16)
            rrb = self.sb(st, "rrb", [128, TB], F32)
            rib = self.sb(st, "rib", [128, TB], F32)
            gt = [self.sb(st, "gt%d" % i, [128, TB], BF16) for i in range(3)]
            tm = [self.sb(st, "tm%d" % i, [128, TB], F32) for i in range(3)]
            sm = [self.sb(st, "sm%d" % i, [128, TB], F32) for i in range(2)]
            xc = [self.sb(st, "xc%d" % i, [128, TB], F32) for i in range(2)]
            xo = [self.sb(st, "xo%d" % i, [128, TB], F32) for i in range(2)]
            S.dma("sync", lambda e: e.dma_start(out=posk.t[:], in_=self.poscol), writes=[posk.b], sig=posk.b)
            cnt = {"kt": 0, "q": 0, "w": 0, "g": 0, "x": 0, "pset": 0}
            ctx_kb = list(range(Sq // 128, NT // 128))

            def load_kt(h):
                k = ktb[0]
                for hf, (a0, a1) in enumerate([(0, HKB * 128), (HKB * 128, NT)]):
                    S.dma("sync", lambda e, k=k, a0=a0, a1=a1, h=h: e.dma_start(out=k.t[:, a0:a1], in_=self.KT[h, :, a0:a1]),
                          writes=[kt_hb[hf]], sig=kt_hb[hf])
                return (k.t, kt_hb)

            vflat = vbuf.t[:].rearrange("p k c -> p (k c)")

            def vbase(dv, kb):
                if dv == 256:
                    return kb * 256
                return kb * 128 if kb < HKB else NKB * 128 + (kb - HKB) * 128

            def load_v(hh, dv, kbs):
                runs = []
                for kb in kbs:
                    if runs and runs[-1][1] == kb and kb != HKB:
                        runs[-1][1] = kb + 1
                    else:
                        runs.append([kb, kb + 1])
                for (a, b) in runs:
                    hf = 0 if a < HKB else 1
                    d0 = vbase(dv, a)
                    S.dma("sync", lambda e, a=a, b=b, dv=dv, hh=hh, d0=d0: e.dma_start(
                        out=vflat[:, d0:d0 + (b - a) * dv], in_=self.VVh[hh, :, a * dv:b * dv]),
                          writes=[v_hb[hf]], sig=v_hb[hf])
                return ((lambda kb, c, dv=dv: vflat[:, vbase(dv, kb) + c * 128: vbase(dv, kb) + (c + 1) * 128]), v_hb)

            def load_q(h, c0, w):
                q = qtl[cnt["q"] % 3]
                cnt["q"] += 1
                S.dma("sync", lambda e, q=q, h=h, c0=c0, w=w: e.dma_start(out=q.t[:, 0:w], in_=self.QT[h, :, c0:c0 + w]),
                      writes=[q.b], sig=q.b)
                return q

            def do_block(c0, w, is_ctx):
                v = 1 if is_ctx else 0
                allkb = ctx_kb if is_ctx else list(range(NKB))
                if is_ctx:
                    a_kb = ctx_kb
                    a_masks = None
                else:
                    band = [((c0 - 128 + 128 * j) % Sq) // 128 for j in range(w // 128 + 2)]
                    a_kb = ctx_kb + band
                    S.dma("sync", lambda e, c0=c0, w=w: e.dma_start(
                        out=posq.t[:, 0:w], in_=self.posrow[0:1, c0:c0 + w].partition_broadcast(128).rearrange("p a b -> p (a b)")),
                          writes=[posq.b], sig=posq.b)
                    a_masks = [None] * len(ctx_kb)
                    for j, kb in enumerate(band):
                        S.op("vector", lambda e, kb=kb, w=w: e.tensor_scalar(out=dtmp.t[:, 0:w], in0=posq.t[:, 0:w], scalar1=posk.t[:, kb:kb + 1],
                                                                             scalar2=None, op0=ALU.subtract),
                             reads=[posq.b, posk.b], writes=[dtmp.b])
                        S.op("vector", lambda e, w=w: e.tensor_tensor(out=dtmp.t[:, 0:w], in0=dtmp.t[:, 0:w], in1=dtmp.t[:, 0:w], op=ALU.mult),
                             reads=[dtmp.b], writes=[dtmp.b])
                        S.op("vector", lambda e, j=j, w=w: e.tensor_scalar(out=mk[j].t[:, 0:w], in0=dtmp.t[:, 0:w], scalar1=16384.5, scalar2=None,
                                                                           op0=ALU.is_le), reads=[dtmp.b], writes=[mk[j].b])
                        a_masks.append(mk[j])
                for kvh in range(2):
                    kt = load_kt(kvh)
                    vv = load_v(kvh, 128, sorted(set(a_kb)))
                    for g in range(4):
                        hq = kvh * 4 + g
                        q = load_q(hq, c0, w)
                        pset = cnt["pset"] % 2
                        cnt["pset"] += 1

                        def fin(c, pso, rz, hq=hq):
                            S.op("vector", lambda e: e.tensor_tensor(out=oT[0].t[:, hq, 0:w], in0=pso.t[:, 0:w], in1=rz.t[:, 0:w], op=ALU.mult),
                                 reads=[pso.b, rz.b], writes=[oT[0].b])
                        self.attn_head(A, q, w, kt, [kb * 128 for kb in a_kb], vv, a_kb, 1, P["negb"].t[:, 0:1], P["negb"].b,
                                       a_masks, (P["esink"].t[:, hq:hq + 1], P["esink"].b), fin, pset)
                for h in range(4):
                    vv = load_v(2 + h, 256, allkb)
                    for comp in range(2):
                        kt = load_kt(2 + 2 * h + comp)
                        q = load_q(8 + 2 * h + comp, c0, w)
                        pset = cnt["pset"] % 2
                        cnt["pset"] += 1

                        def fin(c, pso, rz, comp=comp):
                            S.op("vector", lambda e: e.tensor_tensor(out=o12[comp].t[:, c, 0:w], in0=pso.t[:, 0:w], in1=rz.t[:, 0:w], op=ALU.mult),
                                 reads=[pso.b, rz.b], writes=[o12[comp].b])
                        self.attn_head(A, q, w, kt, [kb * 128 for kb in allkb], vv, allkb, 2, P["negb"].t[:, 1:2], P["negb"].b,
                                       None, None, fin, pset)
                    S.op("vector", lambda e: e.scalar_tensor_tensor(out=obd.t[:, :, 0:w], in0=o12[1].t[:, :, 0:w], scalar=P["neglam"].t[:, 0:1],
                                                                    in1=o12[0].t[:, :, 0:w], op0=ALU.mult, op1=ALU.add),
                         reads=[o12[0].b, o12[1].b, P["neglam"].b], writes=[obd.b])
                    S.op("scalar", lambda e: e.activation(out=obq.t[:, :, 0:w], in_=obd.t[:, :, 0:w], func=AF.Square), reads=[obd.b], writes=[obq.b])
                    psn = self.psb(7)
                    for c in range(2):
                        S.op("tensor", lambda e, c=c: e.matmul(psn.t[:, 0:w], lhsT=P["ones_b"].t[:], rhs=obq.t[:, c, 0:w], start=(c == 0), stop=(c == 1)),
                             reads=[P["ones_b"].b, obq.b], writes=[psn.b])
                    S.op("scalar", lambda e: e.activation(out=rrb.t[:, 0:w], in_=psn.t[:, 0:w], func=AF.Sqrt, bias=P["epsc"].t[:, 0:1], scale=1.0 / 256),
                         reads=[psn.b, P["epsc"].b], writes=[rrb.b])
                    S.op("vector", lambda e: e.reciprocal(out=rib.t[:, 0:w], in_=rrb.t[:, 0:w]), reads=[rrb.b], writes=[rib.b])
                    for c in range(2):
                        S.op("vector", lambda e, c=c, h=h: e.scalar_tensor_tensor(out=oT[1].t[:, 2 * h + c, 0:w], in0=obd.t[:, c, 0:w],
                                                                                 scalar=P["sgT"].t[:, c:c + 1], in1=rib.t[:, 0:w], op0=ALU.mult, op1=ALU.mult),
                             reads=[obd.b, P["sgT"].b, rib.b], writes=[oT[1].b])
                for kvh in range(2):
                    kt = load_kt(10 + kvh)
                    vv = load_v(6 + kvh, 128, allkb)
                    for g in range(4):
                        hq = kvh * 4 + g
                        q = load_q(16 + hq, c0, w)
                        pset = cnt["pset"] % 2
                        cnt["pset"] += 1

                        def fin(c, pso, rz, hq=hq):
                            S.op("vector", lambda e: e.tensor_tensor(out=oT[2].t[:, hq, 0:w], in0=pso.t[:, 0:w], in1=rz.t[:, 0:w], op=ALU.mult),
                                 reads=[pso.b, rz.b], writes=[oT[2].b])
                        self.attn_head(A, q, w, kt, [kb * 128 for kb in allkb], vv, allkb, 1, P["negb"].t[:, 2:3], P["negb"].b,
                                       None, None, fin, pset)
                for pn in range(D // 512 if D >= 512 else 1):
                    pw = min(512, D)
                    wps = [wsl[0], wsl[0], wsl[1]]
                    koff = [0, 8, 0]
                    for br in range(3):
                        self.load_panel(wps[br], self.w_branch[l, br], 0, 1024, pn * 512, pw, k_off=koff[br])
                    for j in range(pw // 128):
                        n = pn * 4 + j
                        tms = []
                        for br in range(3):
                            ps = self.psb(br + 3 * (n % 2))
                            for kc in range(8):
                                S.op("tensor", lambda e, ps=ps, br=br, kc=kc, j=j: e.matmul(
                                    ps.t[:, 0:w], lhsT=wps[br].t[:, koff[br] + kc, j * 128:(j + 1) * 128], rhs=oT[br].t[:, kc, 0:w],
                                    start=(kc == 0), stop=(kc == 7)), reads=[wps[br].b, oT[br].b], writes=[ps.b])
                            g_t = gt[cnt["g"] % 3]
                            t_t = tm[cnt["g"] % 3]
                            cnt["g"] += 1
                            r0 = br * D + n * 128
                            S.dma("sync", lambda e, g_t=g_t, r0=r0: e.dma_start(out=g_t.t[:, 0:w], in_=self.GT[r0:r0 + 128, c0:c0 + w]),
                                  writes=[g_t.b], sig=g_t.b)
                            S.op("vector", lambda e, ps=ps, g_t=g_t, t_t=t_t: e.tensor_tensor(out=t_t.t[:, 0:w], in0=ps.t[:, 0:w], in1=g_t.t[:, 0:w], op=ALU.mult),
                                 reads=[ps.b, g_t.b], writes=[t_t.b])
                            tms.append(t_t)
                        s_t = sm[n % 2]
                        S.op("gpsimd", lambda e, s_t=s_t, tms=tms: e.tensor_tensor(out=s_t.t[:, 0:w], in0=tms[0].t[:, 0:w], in1=tms[1].t[:, 0:w], op=ALU.add),
                             reads=[tms[0].b, tms[1].b], writes=[s_t.b])
                        S.op("gpsimd", lambda e, s_t=s_t, tms=tms, n=n: e.tensor_tensor(out=yT.t[:, n, 0:w], in0=s_t.t[:, 0:w], in1=tms[2].t[:, 0:w], op=ALU.add),
                             reads=[s_t.b, tms[2].b], writes=[yT.b])
                for pn in range(D // 512 if D >= 512 else 1):
                    pw = min(512, D)
                    wp = wsl[cnt["w"] % 2]
                    cnt["w"] += 1
                    self.load_panel(wp, self.w_out[l], 0, D, pn * 512, pw)
                    for j in range(pw // 128):
                        n = pn * 4 + j
                        ps = self.psb(6 + (n % 2))
                        for kc in range(DC):
                            S.op("tensor", lambda e, ps=ps, wp=wp, kc=kc, j=j: e.matmul(
                                ps.t[:, 0:w], lhsT=wp.t[:, kc, j * 128:(j + 1) * 128], rhs=yT.t[:, kc, 0:w],
                                start=(kc == 0), stop=(kc == DC - 1)), reads=[wp.b, yT.b], writes=[ps.b])
                        x_t = xc[cnt["x"] % 2]
                        o_t = xo[cnt["x"] % 2]
                        cnt["x"] += 1
                        S.dma("sync", lambda e, x_t=x_t, n=n: e.dma_start(out=x_t.t[:, 0:w], in_=xin[n * 128:(n + 1) * 128, c0:c0 + w]),
                              writes=[x_t.b], sig=x_t.b)
                        S.op("vector", lambda e, ps=ps, x_t=x_t, o_t=o_t, n=n, v=v: e.scalar_tensor_tensor(
                            out=o_t.t[:, 0:w], in0=ps.t[:, 0:w], scalar=P["modG1"].t[:, v, n:n + 1], in1=x_t.t[:, 0:w],
                            op0=ALU.mult, op1=ALU.add), reads=[ps.b, x_t.b, P["modG1"].b], writes=[o_t.b])
                        S.dma("sync", lambda e, o_t=o_t, n=n: e.dma_start(out=self.X1T[n * 128:(n + 1) * 128, c0:c0 + w], in_=o_t.t[:, 0:w]),
                              reads=[o_t.b], sig=o_t.b)
            for blk in own_blocks:
                do_block(*blk)
            S.flush()

    def stage_ffn(self, l, own_blocks, xout):
        cfg, S, nc, P = self.cfg, self.S, self.nc, self.P
        D, DC, TB, NE = cfg.D, cfg.DC, cfg.TB, cfg.NE
        moe = (self.lsem % 2 == 1)
        li = 0
        DFF = cfg.DFFE if moe else cfg.DFF
        NF = DFF // 128
        self.begin_stage()
        with contextlib.ExitStack() as st:
            xa = self.sb(st, "xa", [128, DC, TB], F32)
            h2 = self.sb(st, "h2", [128, DC, TB], BF16)
            u = self.sb(st, "u", [128, max(NF, DC), TB], BF16)
            nt = self.norm_tiles(st, sq=u)
            wsl = [self.sb(st, "wf%d" % i, [128, 16, 512], BF16) for i in range(3)]
            sa = [self.sb(st, "sa%d" % i, [128, TB], F32) for i in range(2)]
            xc = [self.sb(st, "xc%d" % i, [128, TB], F32) for i in range(1)]
            xo = [self.sb(st, "xo%d" % i, [128, TB], F32) for i in range(1)]
            tw = [self.sb(st, "tw%d" % i, [128, TB], F32) for i in range(1)]
            cnt = {"w": 0, "ab": 0, "x": 0, "t": 0}
            if moe:
                wr = self.sb(st, "wr", [128, DC, NE], F32)
                S.dma("sync", lambda e: e.dma_start(out=wr.t[:], in_=self.moe_router[li].rearrange("(dc p) e -> p dc e", p=128)),
                      writes=[wr.b], sig=wr.b)
                lgT = self.sb(st, "lgT", [NE, TB], F32)
                lg = self.sb(st, "lg", [128, NE], F32)
                m8 = self.sb(st, "m8", [128, 8], F32)
                msk = self.sb(st, "msk", [128, NE], F32)
                ex = self.sb(st, "ex", [128, NE], F32)
                nv1 = self.sb(st, "nv1", [128, 1], F32)
                den = self.sb(st, "den", [128, 1], F32)
                wt = self.sb(st, "wt", [128, NE], F32)
                wtb = self.sb(st, "wtb", [128, 128], F32)
                wtbc = self.sb(st, "wtbc", [128, NE, TB], BF16)

            def ffn_expert(w, w1, w3, w2, e_idx, final_out):
                for fp in range(0, DFF, 512):
                    pw = min(512, DFF - fp)
                    wa = wsl[cnt["w"] % 3]
                    cnt["w"] += 1
                    wb = wsl[cnt["w"] % 3]
                    cnt["w"] += 1
                    self.load_panel(wa, w1, 0, D, fp, pw)
                    self.load_panel(wb, w3, 0, D, fp, pw)
                    for j in range(pw // 128):
                        f = fp // 128 + j
                        pa = self.psb(cnt["ab"] % 2)
                        pb = self.psb(2 + cnt["ab"] % 2)
                        s_t = sa[cnt["ab"] % 2]
                        cnt["ab"] += 1
                        for kc in range(DC):
                            S.op("tensor", lambda e, pa=pa, wa=wa, kc=kc, j=j: e.matmul(
                                pa.t[:, 0:w], lhsT=wa.t[:, kc, j * 128:(j + 1) * 128], rhs=h2.t[:, kc, 0:w],
                                start=(kc == 0), stop=(kc == DC - 1)), reads=[wa.b, h2.b], writes=[pa.b])
                        for kc in range(DC):
                            S.op("tensor", lambda e, pb=pb, wb=wb, kc=kc, j=j: e.matmul(
                                pb.t[:, 0:w], lhsT=wb.t[:, kc, j * 128:(j + 1) * 128], rhs=h2.t[:, kc, 0:w],
                                start=(kc == 0), stop=(kc == DC - 1)), reads=[wb.b, h2.b], writes=[pb.b])
                        S.op("scalar", lambda e, pa=pa, s_t=s_t: e.activation(out=s_t.t[:, 0:w], in_=pa.t[:, 0:w], func=AF.Silu),
                             reads=[pa.b], writes=[s_t.b])
                        S.op("vector", lambda e, pb=pb, s_t=s_t, f=f: e.tensor_tensor(out=u.t[:, f, 0:w], in0=pb.t[:, 0:w], in1=s_t.t[:, 0:w], op=ALU.mult),
                             reads=[pb.b, s_t.b], writes=[u.b])
                for ng in range(0, D, 512):
                    pw = min(512, D - ng)
                    nj = pw // 128
                    pso = [self.psb(4 + j) for j in range(nj)]
                    for f0 in range(0, NF, 16):
                        f1 = min(NF, f0 + 16)
                        wp = wsl[cnt["w"] % 3]
                        cnt["w"] += 1
                        self.load_panel(wp, w2, f0 * 128, (f1 - f0) * 128, ng, pw)
                        for fi in range(f0, f1):
                            for j in range(nj):
                                S.op("tensor", lambda e, wp=wp, fi=fi, f0=f0, j=j: e.matmul(
                                    pso[j].t[:, 0:w], lhsT=wp.t[:, fi - f0, j * 128:(j + 1) * 128], rhs=u.t[:, fi, 0:w],
                                    start=(fi == 0), stop=(fi == NF - 1)), reads=[wp.b, u.b], writes=[pso[j].b])
                    for j in range(nj):
                        n = ng // 128 + j
                        if e_idx is None:
                            final_out(n, pso[j], True)
                        elif e_idx == 0:
                            S.op("vector", lambda e, j=j, n=n: e.tensor_tensor(out=xa.t[:, n, 0:w], in0=pso[j].t[:, 0:w], in1=wtbc.t[:, 0, 0:w], op=ALU.mult),
                                 reads=[pso[j].b, wtbc.b], writes=[xa.b])
                        else:
                            t_t = tw[0]
                            cnt["t"] += 1
                            S.op("vector", lambda e, j=j, t_t=t_t, e_idx=e_idx: e.tensor_tensor(out=t_t.t[:, 0:w], in0=pso[j].t[:, 0:w], in1=wtbc.t[:, e_idx, 0:w], op=ALU.mult),
                                 reads=[pso[j].b, wtbc.b], writes=[t_t.b])
                            S.op("gpsimd", lambda e, n=n, t_t=t_t: e.tensor_tensor(out=xa.t[:, n, 0:w], in0=xa.t[:, n, 0:w], in1=t_t.t[:, 0:w], op=ALU.add),
                                 reads=[xa.b, t_t.b], writes=[xa.b])

            def do_block(c0, w, is_ctx):
                v = 1 if is_ctx else 0

                def final_out(n, src, src_is_psum, c0=c0, w=w, v=v):
                    x_t = xc[0]
                    o_t = xo[0]
                    cnt["x"] += 1
                    S.dma("sync", lambda e: e.dma_start(out=x_t.t[:, 0:w], in_=self.X1T[n * 128:(n + 1) * 128, c0:c0 + w]),
                          writes=[x_t.b], sig=x_t.b)
                    if src_is_psum:
                        S.op("vector", lambda e: e.scalar_tensor_tensor(out=o_t.t[:, 0:w], in0=src.t[:, 0:w], scalar=P["modG2"].t[:, v, n:n + 1],
                                                                        in1=x_t.t[:, 0:w], op0=ALU.mult, op1=ALU.add),
                             reads=[src.b, x_t.b, P["modG2"].b], writes=[o_t.b])
                    else:
                        S.op("vector", lambda e: e.scalar_tensor_tensor(out=o_t.t[:, 0:w], in0=src.t[:, n, 0:w], scalar=P["modG2"].t[:, v, n:n + 1],
                                                                        in1=x_t.t[:, 0:w], op0=ALU.mult, op1=ALU.add),
                             reads=[src.b, x_t.b, P["modG2"].b], writes=[o_t.b])
                    oc0 = self.xmap(c0)
                    S.dma("sync", lambda e: e.dma_start(out=xout[n * 128:(n + 1) * 128, oc0:oc0 + w], in_=o_t.t[:, 0:w]),
                          reads=[o_t.b], sig=o_t.b)

                self.load_xblk(xa, self.X1T, c0, w)
                if not moe:
                    self.modnorm(nt, xa, w, "modA2", "modB2", v, h2)
                    ffn_expert(w, self.dense_w1[li], self.dense_w3[li], self.dense_w2[li], None, final_out)
                    return
                psr = self.psb(6)

                def hook(dc, t2):
                    S.op("tensor", lambda e: e.matmul(psr.t[0:NE, 0:w], lhsT=wr.t[:, dc, :], rhs=t2.t[:, 0:w], start=(dc == 0), stop=(dc == DC - 1)),
                         reads=[wr.b, t2.b], writes=[psr.b])
                self.modnorm(nt, xa, w, "modA2", "modB2", v, h2, hook=hook)
                S.op("vector", lambda e: e.tensor_copy(out=lgT.t[:, 0:w], in_=psr.t[0:NE, 0:w]), reads=[psr.b], writes=[lgT.b])
                for sub in range(w // 128):
                    pst = self.psb(5)
                    S.op("tensor", lambda e, sub=sub: e.matmul(pst.t[:, 0:NE], lhsT=lgT.t[0:NE, sub * 128:(sub + 1) * 128],
                                                               rhs=P["ident_f"].t[0:NE, 0:NE], start=True, stop=True),
                         reads=[lgT.b, P["ident_f"].b], writes=[pst.b])
                    S.op("vector", lambda e: e.tensor_copy(out=lg.t[:], in_=pst.t[:, 0:NE]), reads=[pst.b], writes=[lg.b])
                    S.op("vector", lambda e: e.max(out=m8.t[:], in_=lg.t[:]), reads=[lg.b], writes=[m8.b])
                    S.op("vector", lambda e: e.tensor_scalar(out=msk.t[:], in0=lg.t[:], scalar1=m8.t[:, 1:2], scalar2=None, op0=ALU.is_ge),
                         reads=[lg.b, m8.b], writes=[msk.b])
                    S.op("vector", lambda e: e.tensor_scalar(out=nv1.t[:], in0=m8.t[:, 0:1], scalar1=-1.0, scalar2=None, op0=ALU.mult),
                         reads=[m8.b], writes=[nv1.b])
                    S.op("scalar", lambda e: e.activation(out=ex.t[:], in_=lg.t[:], func=AF.Exp, bias=nv1.t[:, 0:1], scale=1.0),
                         reads=[lg.b, nv1.b], writes=[ex.b])
                    S.op("scalar", lambda e: e.activation(out=den.t[:], in_=m8.t[:, 1:2], func=AF.Exp, bias=nv1.t[:, 0:1], scale=1.0),
                         reads=[m8.b, nv1.b], writes=[den.b])
                    S.op("vector", lambda e: e.tensor_scalar(out=den.t[:], in0=den.t[:], scalar1=1.0, scalar2=None, op0=ALU.add),
                         reads=[den.b], writes=[den.b])
                    S.op("vector", lambda e: e.reciprocal(out=den.t[:], in_=den.t[:]), reads=[den.b], writes=[den.b])
                    S.op("vector", lambda e: e.scalar_tensor_tensor(out=wt.t[:], in0=ex.t[:], scalar=den.t[:, 0:1], in1=msk.t[:], op0=ALU.mult, op1=ALU.mult),
                         reads=[ex.b, den.b, msk.b], writes=[wt.b])
                    for ei in range(NE):
                        S.op("vector", lambda e, ei=ei: e.tensor_scalar(out=wtb.t[:], in0=P["ones_f"].t[:], scalar1=wt.t[:, ei:ei + 1], scalar2=None, op0=ALU.mult),
                             reads=[wt.b, P["ones_f"].b], writes=[wtb.b])
                        psb_ = self.psb(4)
                        S.op("tensor", lambda e, psb_=psb_: e.matmul(psb_.t[:, 0:128], lhsT=wtb.t[:], rhs=P["ident_f"].t[:], start=True, stop=True),
                             reads=[wtb.b, P["ident_f"].b], writes=[psb_.b])
                        S.op("vector", lambda e, ei=ei, sub=sub, psb_=psb_: e.tensor_copy(out=wtbc.t[:, ei, sub * 128:(sub + 1) * 128], in_=psb_.t[:, 0:128]),
                             reads=[psb_.b], writes=[wtbc.b])
                for ei in range(NE):
                    ffn_expert(w, self.moe_w1[ei], self.moe_w3[ei], self.moe_w2[ei], ei, final_out)
                for n in range(DC):
                    final_out(n, xa, False)
            for blk in own_blocks:
                do_block(*blk)
            S.flush()


def make_consts():
    c = np.zeros((128, 384), np.float32)
    c[:, 0:128] = np.eye(128, dtype=np.float32)
    prot = np.zeros((128, 128), np.float32)
    for d in range(128):
        partner = d + 32 if (d % 64) < 32 else d - 32
        prot[partner, d] = 1.0
    c[:, 128:256] = prot
    c[:, 256:384] = 1.0
    return c


def core_inputs(cfg, inputs, core, shared):
    b, q = core // 4, core % 4
    S, CTX, NT, OWN = cfg.S, cfg.CTX, cfg.NT, cfg.OWN
    x = np.asarray(inputs["x"])[b]
    ctx = np.asarray(inputs["ctx"])[b]
    tok = (q * OWN + np.arange(S)) % S
    xT = np.ascontiguousarray(np.concatenate([x[tok].T, ctx.T], axis=1), dtype=np.float32)
    row = (tok // cfg.GRID_W).astype(np.float32)
    colp = (tok % cfg.GRID_W).astype(np.float32)
    inv = (10000.0 ** (-np.arange(32, dtype=np.float32) / 32)).astype(np.float32)
    cosT = np.ones((128, NT), np.float32)
    sinT = np.zeros((128, NT), np.float32)
    for a, pos in enumerate([row, colp]):
        ang = (pos[None, :] * inv[:, None]).astype(np.float32)
        c, s = np.cos(ang).astype(np.float32), np.sin(ang).astype(np.float32)
        cosT[a * 64:a * 64 + 32, :S] = c
        cosT[a * 64 + 32:a * 64 + 64, :S] = c
        sinT[a * 64:a * 64 + 32, :S] = -s
        sinT[a * 64 + 32:a * 64 + 64, :S] = s
    pos = np.concatenate([tok.astype(np.float32), np.full((CTX,), -1.0e6, np.float32)])
    m = {
        "xT": xT, "cosT": cosT, "sinT": sinT, "posrow": pos[None, :].copy(),
        "poscol": np.ascontiguousarray(pos.reshape(NT // 128, 128).T),
        "consts": shared["consts"],
        "cvec": np.ascontiguousarray(np.stack([np.asarray(inputs["c"])[b], np.asarray(inputs["c_ctx"])], axis=0), dtype=np.float32),
    }
    m.update(shared["w"])
    return m


def tile_w(W):
    W = np.asarray(W, dtype=np.float32)
    K, N = W.shape[-2:]
    lead = W.shape[:-2]
    NP = (N + 511) // 512
    if NP * 512 != N:
        Wp = np.zeros(lead + (K, NP * 512), np.float32)
        Wp[..., :N] = W
        W = Wp
    W = W.reshape(lead + (K // 128, 128, NP, 512))
    nl = len(lead)
    perm = tuple(range(nl)) + (nl + 2, nl + 1, nl + 0, nl + 3)
    return np.ascontiguousarray(W.transpose(perm))


def shared_inputs(cfg, inputs, lw):
    def lay(a):
        a = np.asarray(a, dtype=np.float32)
        return a if lw is None else a[lw:lw + 1]
    w = {}
    for k in ["b_mod", "norm1", "norm2", "a_sink", "b_subln"]:
        w[k] = lay(inputs[k])
    for k in ["w_mod", "w_in", "w_branch", "w_out"]:
        w[k] = tile_w(lay(inputs[k]))
    L = w["b_mod"].shape[0]
    w["qk_gain"] = lay(inputs["qk_gain"]).reshape(L, 6 * 128)
    w["b_lambda"] = lay(inputs["b_lambda"]).reshape(L, 4 * 128)
    need_dense = lw in (None, 0)
    need_moe = lw in (None, 1)
    for k in ["dense_w1", "dense_w3", "dense_w2"]:
        w[k] = tile_w(inputs[k]) if need_dense else np.zeros((1, 1, 128, 1, 512), np.float32)
    w["moe_router"] = np.asarray(inputs["moe_router"], dtype=np.float32)
    for k in ["moe_w1", "moe_w3", "moe_w2"]:
        if need_moe:
            a = np.asarray(inputs[k], dtype=np.float32)
            a = a.reshape(a.shape[1:]) if a.shape[0] == 1 else a
            w[k] = tile_w(a)
        else:
            w[k] = np.zeros((cfg.NE, 1, 128, 1, 512), np.float32)
    return {"consts": make_consts(), "w": w}


_NC_CACHE = {}


def _program(cfg, debug=False):
    key = (cfg.D, cfg.S, cfg.DFF, cfg.DFFE, cfg.TB, cfg.DEPTH, cfg.mode, debug)
    if key not in _NC_CACHE:
        bld = Builder(cfg)
        bld.debug = debug
        _NC_CACHE[key] = bld.build()
    return _NC_CACHE[key]


def run(cfg, inputs, trace=False, debug=False):
    nc = _program(cfg, debug)
    sh = shared_inputs(cfg, inputs, None)
    in_maps = [core_inputs(cfg, inputs, c, sh) for c in range(8)]
    res = run_bass_kernel_spmd(nc, in_maps, core_ids=list(range(8)), **({"trace": True} if trace else {}))
    B = np.asarray(inputs["x"]).shape[0]
    out = np.zeros((B, cfg.S, cfg.D), np.float32)
    for c in range(8):
        b, q = c // 4, c % 4
        out[b, q * cfg.OWN:(q + 1) * cfg.OWN, :] = res.results[c]["yT"].T
    return out, res


def run_split(inputs, cfg_kw=None, trace=False):
    cfg_kw = cfg_kw or {}
    cfg0 = Cfg(mode="L0", **cfg_kw)
    nc0 = _program(cfg0)
    sh0 = shared_inputs(cfg0, inputs, 0)
    tk = {"trace": True} if trace else {}
    res0 = run_bass_kernel_spmd(nc0, [core_inputs(cfg0, inputs, c, sh0) for c in range(8)], core_ids=list(range(8)), **tk)
    x1 = np.zeros(np.asarray(inputs["x"]).shape, np.float32)
    ctx1 = np.zeros(np.asarray(inputs["ctx"]).shape, np.float32)
    OWN = cfg0.OWN
    for c in range(8):
        b, q = c // 4, c % 4
        y = res0.results[c]["yT"]
        x1[b, q * OWN:(q + 1) * OWN, :] = y[:, :OWN].T
        if q == 0:
            ctx1[b] = y[:, OWN:].T
    inputs1 = dict(inputs)
    inputs1["x"], inputs1["ctx"] = x1, ctx1
    cfg1 = Cfg(mode="L1", **cfg_kw)
    nc1 = _program(cfg1)
    sh1 = shared_inputs(cfg1, inputs, 1)
    res1 = run_bass_kernel_spmd(nc1, [core_inputs(cfg1, inputs1, c, sh1) for c in range(8)], core_ids=list(range(8)), **tk)
    out = np.zeros(x1.shape, np.float32)
    for c in range(8):
        b, q = c // 4, c % 4
        out[b, q * OWN:(q + 1) * OWN, :] = res1.results[c]["yT"].T
    return out, (res0, res1)


SPLIT = False


def kernel(**inputs):
    if SPLIT:
        out, _ = run_split(inputs)
        return out
    cfg = Cfg()
    out, _ = run(cfg, inputs)
    return out
```

```python
import contextlib
import math
import numpy as np
import concourse.bass as bass
import concourse.mybir as mybir
from concourse.bass_utils import run_bass_kernel_spmd

F32 = mybir.dt.float32
BF16 = mybir.dt.bfloat16
AF = mybir.ActivationFunctionType
ALU = mybir.AluOpType
AX = mybir.AxisListType

ENGS = ["sync", "scalar", "gpsimd", "vector", "tensor"]
SAME_ENGINE_SYNC = {"scalar": True, "vector": True, "gpsimd": True, "tensor": False, "sync": False}
N_DMA_SEMS = 72


class Cfg:
    def __init__(self, D=2048, S=8192, CTX=256, DFF=5632, DFFE=7168, NE=8, GRID_W=64, TB=512, DEPTH=2, mode="fused"):
        self.mode = mode
        self.D, self.S, self.CTX, self.DFF, self.DFFE, self.NE, self.GRID_W, self.TB, self.DEPTH = \
            D, S, CTX, DFF, DFFE, NE, GRID_W, TB, DEPTH
        self.DC = D // 128
        self.NT = S + CTX
        self.OWN = S // 4
        self.HD = 128
        self.IN_COLS = 6144 + 3 * D
        self.EPS = 1e-6
        self.segs = [("aq", 1024, "q", 0, 0), ("ak", 256, "k", 1, 0), ("av", 256, "v", -1, 0),
                     ("bq", 1024, "q", 2, 8), ("bk", 1024, "k", 3, 2), ("bv", 1024, "v", -1, 256),
                     ("cq", 1024, "q", 4, 16), ("ck", 256, "k", 5, 10), ("cv", 256, "v", -1, 1280),
                     ("ga", D, "g", -1, 0), ("gb", D, "g", -1, D), ("gc", D, "g", -1, 2 * D)]


class Buf:
    __slots__ = ("name", "last_write", "readers", "pool_idx", "dma_total")

    def __init__(self, name):
        self.name = name
        self.last_write = None
        self.readers = {}
        self.pool_idx = None
        self.dma_total = 0


class Op:
    __slots__ = ("eng", "fn", "deps", "is_dma", "sig", "sigval", "signal", "idx", "pool_idx")


class Sched:
    def __init__(self, nc, st):
        self.nc = nc
        self.ops = []
        self.flushed = 0
        self.esem = {e: st.enter_context(nc.semaphore("es_" + e)) for e in ENGS}
        self.dsem = [st.enter_context(nc.semaphore("ds_%d" % i)) for i in range(N_DMA_SEMS)]
        self.dtotal = [0] * N_DMA_SEMS
        self.ecount = {e: 0 for e in ENGS}
        self.known = {e: {} for e in ENGS}
        self.stage_bufs = []
        self.pool_next = 0
        self.pool_used = set()

    def buf(self, name):
        b = Buf(name)
        self.stage_bufs.append(b)
        return b

    def _pool(self, b):
        if b.pool_idx is None:
            for _ in range(N_DMA_SEMS):
                i = self.pool_next
                self.pool_next = (self.pool_next + 1) % N_DMA_SEMS
                if i not in self.pool_used:
                    break
            else:
                raise RuntimeError("out of dma sems")
            self.pool_used.add(i)
            b.pool_idx = i
            b.dma_total = self.dtotal[i]
        return b.pool_idx

    def _add(self, eng, fn, reads, writes, is_dma=False, sig=None):
        op = Op()
        op.eng, op.fn, op.is_dma, op.sig = eng, fn, is_dma, sig
        op.sigval, op.signal, op.idx, op.pool_idx = None, False, len(self.ops), None
        deps = set()
        for b in reads:
            if b.last_write is not None:
                deps.add(b.last_write)
        for b in writes:
            if b.last_write is not None:
                lw = self.ops[b.last_write]
                if not (is_dma and lw.is_dma and lw.sig is sig):
                    deps.add(b.last_write)
            for r in b.readers.values():
                deps.add(r)
        op.deps = deps
        self.ops.append(op)
        key = ("dma", op.idx) if is_dma else eng
        for b in reads:
            b.readers[key] = op.idx
        for b in writes:
            b.last_write = op.idx
            b.readers = {}
        if is_dma:
            op.pool_idx = self._pool(sig)
            sig.dma_total += 16
            self.dtotal[op.pool_idx] = sig.dma_total
            op.sigval = sig.dma_total
        return op

    def op(self, eng, fn, reads=(), writes=()):
        return self._add(eng, fn, list(reads), list(writes))

    def dma(self, eng, fn, reads=(), writes=(), sig=None):
        return self._add(eng, fn, list(reads), list(writes), True, sig)

    def _event(self, o):
        if o.is_dma:
            return self.dsem[o.pool_idx], o.sigval
        return self.esem[o.eng], o.sigval

    def flush(self, final=False):
        nc = self.nc
        ops = self.ops
        new = ops[self.flushed:]
        base = self.flushed
        per_eng = {e: [] for e in ENGS}
        for o in new:
            per_eng[o.eng].append(o)
        for o in new:
            for d in o.deps:
                od = ops[d]
                if d >= base and not od.is_dma:
                    if od.eng != o.eng or SAME_ENGINE_SYNC[o.eng] or o.is_dma:
                        od.signal = True
        for e in ENGS:
            for o in reversed(per_eng[e]):
                if not o.is_dma:
                    o.signal = True
                    break
        for o in new:
            if not o.is_dma and o.signal:
                self.ecount[o.eng] += 1
                o.sigval = self.ecount[o.eng]
        finals = []
        for e in ENGS:
            if self.ecount[e] > 0:
                finals.append((self.esem[e], self.ecount[e]))
        for i in range(N_DMA_SEMS):
            if self.dtotal[i] > 0:
                finals.append((self.dsem[i], self.dtotal[i]))

        with nc.Block() as block:
            def make(engname):
                def body(eng):
                    known = self.known[engname]
                    for o in per_eng[engname]:
                        need = {}
                        for d in o.deps:
                            if d < base:
                                continue
                            od = ops[d]
                            if (not od.is_dma) and od.eng == engname and not (SAME_ENGINE_SYNC[engname] or o.is_dma):
                                continue
                            s, v = self._event(od)
                            if need.get(id(s), (None, 0))[1] < v:
                                need[id(s)] = (s, v)
                        for sid, (s, v) in need.items():
                            if known.get(sid, 0) < v:
                                eng.wait_ge(s, v)
                                known[sid] = v
                        ins = o.fn(eng)
                        if o.is_dma:
                            ins.then_inc(self.dsem[o.pool_idx], 16)
                        elif o.signal:
                            ins.then_inc(self.esem[engname], 1)
                    for (s, v) in finals:
                        if known.get(id(s), 0) < v:
                            eng.wait_ge(s, v)
                            known[id(s)] = v
                return body

            for e in ENGS:
                getattr(block, e)(make(e))
        self.flushed = len(ops)
        self.stage_bufs = []
        self.pool_used = set()


class TileT:
    __slots__ = ("t", "b")

    def __init__(self, t, b):
        self.t, self.b = t, b


class Builder:
    def __init__(self, cfg):
        self.cfg = cfg

    def sb(self, st, name, shape, dt):
        self.uid = getattr(self, "uid", 0) + 1
        name = "%s_u%d" % (name, self.uid)
        t = st.enter_context(self.nc.sbuf_tensor(name, list(shape), dt))
        return TileT(t, self.S.buf(name))

    def rebuf(self, tiles):
        for t in tiles:
            t.b = self.S.buf(t.b.name)

    def build(self):
        cfg = self.cfg
        D, NT, DC, OWN = cfg.D, cfg.NT, cfg.DC, cfg.OWN
        nc = bass.Bass("TRN2", target_bir_lowering=False)
        self.nc = nc
        L = cfg.DEPTH if cfg.mode == "fused" else 1

        def din(name, shape, dt=F32):
            return nc.dram_tensor(name, list(shape), dt, kind="ExternalInput").ap()

        self.xT = din("xT", [D, NT])
        self.cosT = din("cosT", [128, NT])
        self.sinT = din("sinT", [128, NT])
        self.posrow = din("posrow", [1, NT])
        self.poscol = din("poscol", [128, NT // 128])
        self.consts = din("consts", [128, 384])
        self.cvec = din("cvec", [2, D])
        def tsh(K, N):
            return [(N + 511) // 512, 128, K // 128, 512]

        self.w_mod = din("w_mod", [L] + tsh(D, 6 * D))
        self.b_mod = din("b_mod", [L, 6 * D])
        self.norm1 = din("norm1", [L, D])
        self.norm2 = din("norm2", [L, D])
        self.w_in = din("w_in", [L] + tsh(D, cfg.IN_COLS))
        self.qk_gain = din("qk_gain", [L, 6 * 128])
        self.a_sink = din("a_sink", [L, 8])
        self.b_lambda = din("b_lambda", [L, 4 * 128])
        self.b_subln = din("b_subln", [L, 256])
        self.w_branch = din("w_branch", [L, 3] + tsh(1024, D))
        self.w_out = din("w_out", [L] + tsh(D, D))
        if cfg.mode == "L1":
            self.dense_w1 = din("dense_w1", [1] + tsh(128, 512))
            self.dense_w3 = din("dense_w3", [1] + tsh(128, 512))
            self.dense_w2 = din("dense_w2", [1] + tsh(128, 512))
        else:
            self.dense_w1 = din("dense_w1", [1] + tsh(D, cfg.DFF))
            self.dense_w3 = din("dense_w3", [1] + tsh(D, cfg.DFF))
            self.dense_w2 = din("dense_w2", [1] + tsh(cfg.DFF, D))
        self.moe_router = din("moe_router", [1, D, cfg.NE])
        if cfg.mode == "L0":
            self.moe_w1 = din("moe_w1", [cfg.NE] + tsh(128, 512))
            self.moe_w3 = din("moe_w3", [cfg.NE] + tsh(128, 512))
            self.moe_w2 = din("moe_w2", [cfg.NE] + tsh(128, 512))
        else:
            self.moe_w1 = din("moe_w1", [cfg.NE] + tsh(D, cfg.DFFE))
            self.moe_w3 = din("moe_w3", [cfg.NE] + tsh(D, cfg.DFFE))
            self.moe_w2 = din("moe_w2", [cfg.NE] + tsh(cfg.DFFE, D))
        self.yT = nc.dram_tensor("yT", [D, OWN + (cfg.CTX if cfg.mode == "L0" else 0)], F32, kind="ExternalOutput").ap()
        dbg = getattr(self, "debug", False)
        kw = {"kind": "ExternalOutput"} if dbg else {}
        self.QT = nc.dram_tensor("QT", [24, 128, NT], BF16, **kw).ap()
        self.KT = nc.dram_tensor("KT", [12, 128, NT], BF16, **kw).ap()
        self.VVh = nc.dram_tensor("VVh", [8, 128, (NT // 128) * 256], BF16, **kw).ap()
        self.GT = nc.dram_tensor("GT", [3 * D, NT], BF16, **kw).ap()
        self.X1T = nc.dram_tensor("X1T", [D, NT], F32, **kw).ap()
        self.X2T = nc.dram_tensor("X2T", [D, NT], F32, **kw).ap()

        with contextlib.ExitStack() as st0:
            self.S = Sched(nc, st0)
            S = self.S
            self.ps = [TileT(st0.enter_context(nc.psum_tensor("ps%d" % i, [128, 512], F32)), S.buf("ps%d" % i))
                       for i in range(8)]
            P = {}
            self.P = P
            for name, shape, dt in [("ident_f", [128, 128], F32), ("ones_f", [128, 128], F32),
                                    ("ones_b", [128, 128], BF16), ("prot_b", [128, 128], BF16),
                                    ("modA1", [128, 2, DC], F32), ("modB1", [128, 2, DC], F32), ("modG1", [128, 2, DC], F32),
                                    ("modA2", [128, 2, DC], F32), ("modB2", [128, 2, DC], F32), ("modG2", [128, 2, DC], F32),
                                    ("gainT", [128, 6], F32), ("negb", [128, 3], F32), ("esink", [128, 8], F32),
                                    ("neglam", [128, 1], F32), ("sgT", [128, 2], F32), ("epsc", [128, 1], F32)]:
                P[name] = self.sb(st0, name, shape, dt)
            self.persist = list(P.values())
            self.stage_consts()
            xin = self.xT
            if cfg.mode == "fused":
                plan = [(l, l, l == L - 1, OWN if l == L - 1 else getattr(cfg, 'L0_OWN', cfg.S)) for l in range(L)]
            elif cfg.mode == "L0":
                plan = [(0, 0, False, OWN)]
            else:
                plan = [(0, 1, True, OWN)]
            for (l, lsem, last, own_lat) in plan:
                self.lsem = lsem
                blocks_all = [(c0, cfg.TB, False) for c0 in range(0, cfg.S, cfg.TB)]
                cw = min(cfg.TB, cfg.CTX)
                ctx_blocks = [(cfg.S + c0, cw, True) for c0 in range(0, cfg.CTX, cw)]
                own_blocks = [b for b in blocks_all if b[0] < own_lat] + ([] if last else ctx_blocks)
                self.stage_mod(l)
                self.stage_proj(l, xin, blocks_all + ctx_blocks, own_lat, with_ctx=not last)
                self.stage_attn(l, xin, own_blocks)
                if cfg.mode == "L0":
                    xout, xmap = self.yT, (lambda c0: c0 if c0 < cfg.S else OWN + (c0 - cfg.S))
                else:
                    xout, xmap = (self.yT if last else self.X2T), (lambda c0: c0)
                self.xmap = xmap
                self.stage_ffn(l, own_blocks, xout)
                xin = self.X2T
            S.flush(final=True)
        return nc

    def psb(self, i):
        return self.ps[i]

    def begin_stage(self):
        self.rebuf(self.persist)
        self.rebuf(self.ps)

    def row_to_cols(self, st, row_ap, n, name):
        S, nc = self.S, self.nc
        rowt = self.sb(st, name + "_row", [1, n * 128], F32)
        out = self.sb(st, name + "_cols", [128, n], F32)
        S.dma("sync", lambda e: e.dma_start(out=rowt.t[:], in_=row_ap), writes=[rowt.b], sig=rowt.b)
        ps = self.psb(7)
        one = self.P["ones_f"]
        for j in range(n):
            S.op("tensor", lambda e, j=j: e.matmul(ps.t[:, j:j + 1], lhsT=rowt.t[0:1, j * 128:(j + 1) * 128],
                                                   rhs=one.t[0:1, 0:1], start=True, stop=True),
                 reads=[rowt.b, one.b], writes=[ps.b])
        S.op("vector", lambda e: e.tensor_copy(out=out.t[:], in_=ps.t[:, 0:n]), reads=[ps.b], writes=[out.b])
        return out

    def stage_consts(self):
        S, nc, P = self.S, self.nc, self.P
        with contextlib.ExitStack() as st:
            cst = self.sb(st, "cst", [128, 384], F32)
            S.dma("sync", lambda e: e.dma_start(out=cst.t[:], in_=self.consts), writes=[cst.b], sig=cst.b)
            S.op("vector", lambda e: e.tensor_copy(out=P["ident_f"].t[:], in_=cst.t[:, 0:128]), reads=[cst.b], writes=[P["ident_f"].b])
            S.op("vector", lambda e: e.tensor_copy(out=P["prot_b"].t[:], in_=cst.t[:, 128:256]), reads=[cst.b], writes=[P["prot_b"].b])
            S.op("vector", lambda e: e.tensor_copy(out=P["ones_f"].t[:], in_=cst.t[:, 256:384]), reads=[cst.b], writes=[P["ones_f"].b])
            S.op("vector", lambda e: e.tensor_copy(out=P["ones_b"].t[:], in_=cst.t[:, 256:384]), reads=[cst.b], writes=[P["ones_b"].b])
            S.op("vector", lambda e: e.memset(P["epsc"].t[:], self.cfg.EPS), writes=[P["epsc"].b])
            S.flush()

    def stage_mod(self, l):
        cfg, S, nc, P = self.cfg, self.S, self.nc, self.P
        D, DC = cfg.D, cfg.DC
        lam_init = 0.8 - 0.6 * math.exp(-0.3 * self.lsem)
        self.begin_stage()
        with contextlib.ExitStack() as st:
            sc = self.sb(st, "silc", [128, DC, 2], BF16)
            for v in range(2):
                cc = self.row_to_cols(st, self.cvec[v:v + 1, :], DC, "c%d" % v)
                S.op("scalar", lambda e, cc=cc, v=v: e.activation(out=sc.t[:, :, v], in_=cc.t[:], func=AF.Silu),
                     reads=[cc.b], writes=[sc.b])
            n1 = self.row_to_cols(st, self.norm1[l:l + 1, :], DC, "n1")
            n2 = self.row_to_cols(st, self.norm2[l:l + 1, :], DC, "n2")
            brow = self.sb(st, "brow", [1, 6 * D], BF16)
            for c0 in range(0, 6 * D, 2048):
                c1 = min(6 * D, c0 + 2048)
                S.dma("gpsimd", lambda e, c0=c0, c1=c1: e.dma_start(out=brow.t[0:1, c0:c1], in_=self.b_mod[l:l + 1, c0:c1]),
                      writes=[brow.b], sig=brow.b)
            modT = self.sb(st, "modT", [128, 6 * DC, 2], F32)
            wsl = [self.sb(st, "wm%d" % i, [128, DC, 512], BF16) for i in range(2)]
            psm = self.psb(0)
            npan = 6 * D // 512
            for pn in range(npan):
                w = wsl[pn % 2]
                self.load_panel(w, self.w_mod[l], 0, D, pn * 512, 512)
                for j in range(4):
                    ch = pn * 4 + j
                    for kc in range(DC):
                        S.op("tensor", lambda e, w=w, kc=kc, j=j, ch=ch: e.matmul(
                            psm.t[:, ch * 2:ch * 2 + 2], lhsT=w.t[:, kc, j * 128:(j + 1) * 128], rhs=sc.t[:, kc, :],
                            start=(kc == 0), stop=False), reads=[w.b, sc.b], writes=[psm.b])
                    S.op("tensor", lambda e, ch=ch: e.matmul(
                        psm.t[:, ch * 2:ch * 2 + 2], lhsT=brow.t[0:1, ch * 128:(ch + 1) * 128], rhs=P["ones_b"].t[0:1, 0:2],
                        start=False, stop=True), reads=[brow.b, P["ones_b"].b], writes=[psm.b])
            S.op("vector", lambda e: e.tensor_copy(out=modT.t[:].rearrange("p a b -> p (a b)"), in_=psm.t[:, 0:12 * DC]),
                 reads=[psm.b], writes=[modT.b])
            for v in range(2):
                for (nm, nrm, i_sh, i_sc, i_g, A, B, G) in [(1, n1, 0, 1, 2, "modA1", "modB1", "modG1"),
                                                            (2, n2, 3, 4, 5, "modA2", "modB2", "modG2")]:
                    S.op("vector", lambda e, v=v, nrm=nrm, i_sc=i_sc, A=A: e.scalar_tensor_tensor(
                        out=P[A].t[:, v, :], in0=modT.t[:, i_sc * DC:(i_sc + 1) * DC, v], scalar=1.0, in1=nrm.t[:],
                        op0=ALU.add, op1=ALU.mult), reads=[modT.b, nrm.b], writes=[P[A].b])
                    S.op("vector", lambda e, v=v, i_sh=i_sh, B=B: e.tensor_copy(
                        out=P[B].t[:, v, :], in_=modT.t[:, i_sh * DC:(i_sh + 1) * DC, v]), reads=[modT.b], writes=[P[B].b])
                    S.op("vector", lambda e, v=v, i_g=i_g, G=G: e.tensor_copy(
                        out=P[G].t[:, v, :], in_=modT.t[:, i_g * DC:(i_g + 1) * DC, v]), reads=[modT.b], writes=[P[G].b])
            gb = self.sb(st, "gb", [128, 6, 128], F32)
            S.dma("sync", lambda e: e.dma_start(out=gb.t[:].rearrange("p a b -> p (a b)"),
                                                in_=self.qk_gain[l:l + 1, :].partition_broadcast(128).rearrange("p a b -> p (a b)")),
                  writes=[gb.b], sig=gb.b)
            gm = self.sb(st, "gm", [128, 6], F32)
            S.op("vector", lambda e: e.tensor_reduce(out=gm.t[:], in_=gb.t[:], axis=AX.X, op=ALU.max, apply_absolute_value=True),
                 reads=[gb.b], writes=[gm.b])
            for br in range(3):
                S.op("vector", lambda e, br=br: e.scalar_tensor_tensor(
                    out=P["negb"].t[:, br:br + 1], in0=gm.t[:, 2 * br:2 * br + 1], scalar=-math.sqrt(128.0),
                    in1=gm.t[:, 2 * br + 1:2 * br + 2], op0=ALU.mult, op1=ALU.mult), reads=[gm.b], writes=[P["negb"].b])
            with nc.allow_non_contiguous_dma(reason="tiny transposed param load"):
                S.dma("sync", lambda e: e.dma_start(out=P["gainT"].t[:], in_=self.qk_gain[l, :].rearrange("(g p) -> p g", p=128),
                                                    allow_slow_non_contiguous=True),
                      writes=[P["gainT"].b], sig=P["gainT"].b)
                S.dma("sync", lambda e: e.dma_start(out=P["sgT"].t[:], in_=self.b_subln[l, :].rearrange("(g p) -> p g", p=128),
                                                    allow_slow_non_contiguous=True),
                      writes=[P["sgT"].b], sig=P["sgT"].b)
            S.op("vector", lambda e: e.tensor_scalar(out=P["sgT"].t[:], in0=P["sgT"].t[:], scalar1=(1.0 - lam_init), scalar2=None,
                                                     op0=ALU.mult), reads=[P["sgT"].b], writes=[P["sgT"].b])
            sk = self.sb(st, "sk", [128, 8], F32)
            S.dma("sync", lambda e: e.dma_start(out=sk.t[:], in_=self.a_sink[l:l + 1, :].partition_broadcast(128).rearrange("p a b -> p (a b)")),
                  writes=[sk.b], sig=sk.b)
            S.op("scalar", lambda e: e.activation(out=P["esink"].t[:], in_=sk.t[:], func=AF.Exp, bias=P["negb"].t[:, 0:1], scale=1.0),
                 reads=[sk.b, P["negb"].b], writes=[P["esink"].b])
            lb = self.sb(st, "lb", [128, 4, 128], F32)
            S.dma("sync", lambda e: e.dma_start(out=lb.t[:].rearrange("p a b -> p (a b)"),
                                                in_=self.b_lambda[l:l + 1, :].partition_broadcast(128).rearrange("p a b -> p (a b)")),
                  writes=[lb.b], sig=lb.b)
            pr = self.sb(st, "lpr", [128, 2, 128], F32)
            ss = self.sb(st, "lss", [128, 2], F32)
            ee = self.sb(st, "lee", [128, 2], F32)
            for i in range(2):
                S.op("vector", lambda e, i=i: e.tensor_tensor(out=pr.t[:, i, :], in0=lb.t[:, 2 * i, :], in1=lb.t[:, 2 * i + 1, :], op=ALU.mult),
                     reads=[lb.b], writes=[pr.b])
            S.op("vector", lambda e: e.tensor_reduce(out=ss.t[:], in_=pr.t[:], axis=AX.X, op=ALU.add), reads=[pr.b], writes=[ss.b])
            S.op("scalar", lambda e: e.activation(out=ee.t[:], in_=ss.t[:], func=AF.Exp), reads=[ss.b], writes=[ee.b])
            S.op("vector", lambda e: e.scalar_tensor_tensor(out=P["neglam"].t[:], in0=ee.t[:, 1:2], scalar=-lam_init, in1=ee.t[:, 0:1],
                                                            op0=ALU.add, op1=ALU.subtract), reads=[ee.b], writes=[P["neglam"].b])
            S.flush()

    def load_panel(self, w, wsrc, r0, nrows, c0, ncols, eng="gpsimd", k_off=0):
        S = self.S
        nk = nrows // 128
        k0 = r0 // 128
        pn, coff = c0 // 512, c0 % 512
        assert coff + ncols <= 512
        src = wsrc[pn]
        step = 4
        for a0 in range(0, nk, step):
            a1 = min(nk, a0 + step)
            S.dma(eng, lambda e, a0=a0, a1=a1: e.dma_start(out=w.t[:, k_off + a0:k_off + a1, 0:ncols],
                                                           in_=src[:, k0 + a0:k0 + a1, coff:coff + ncols], max_dma_last_dim=8192),
                  writes=[w.b], sig=w.b)

    def modnorm(self, st_tiles, xblk, w, A, B, v, hT, hook=None):
        cfg, S, P = self.cfg, self.S, self.P
        DC, D = cfg.DC, cfg.D
        sq, rr, rinv, tmp2 = st_tiles["sq"], st_tiles["rr"], st_tiles["rinv"], st_tiles["tmpn"]
        pss = self.psb(7)
        S.op("scalar", lambda e: e.activation(out=sq.t[:, 0:DC, 0:w], in_=xblk.t[:, 0:DC, 0:w], func=AF.Square),
             reads=[xblk.b], writes=[sq.b])
        for dc in range(DC):
            S.op("tensor", lambda e, dc=dc: e.matmul(pss.t[:, 0:w], lhsT=P["ones_b"].t[:], rhs=sq.t[:, dc, 0:w],
                                                     start=(dc == 0), stop=(dc == DC - 1)),
                 reads=[P["ones_b"].b, sq.b], writes=[pss.b])
        S.op("scalar", lambda e: e.activation(out=rr.t[:, 0:w], in_=pss.t[:, 0:w], func=AF.Sqrt, bias=P["epsc"].t[:, 0:1], scale=1.0 / D),
             reads=[pss.b, P["epsc"].b], writes=[rr.b])
        S.op("vector", lambda e: e.reciprocal(out=rinv.t[:, 0:w], in_=rr.t[:, 0:w]), reads=[rr.b], writes=[rinv.b])
        for dc in range(DC):
            t2 = tmp2[dc % 2]
            S.op("vector", lambda e, dc=dc, t2=t2: e.scalar_tensor_tensor(
                out=t2.t[:, 0:w], in0=xblk.t[:, dc, 0:w], scalar=P[A].t[:, v, dc:dc + 1], in1=rinv.t[:, 0:w],
                op0=ALU.mult, op1=ALU.mult), reads=[xblk.b, P[A].b, rinv.b], writes=[t2.b])
            if hook is None:
                S.op("scalar", lambda e, dc=dc, t2=t2: e.activation(out=hT.t[:, dc, 0:w], in_=t2.t[:, 0:w], func=AF.Identity,
                                                                    bias=P[B].t[:, v, dc:dc + 1], scale=1.0),
                     reads=[t2.b, P[B].b], writes=[hT.b])
            else:
                S.op("scalar", lambda e, dc=dc, t2=t2: e.activation(out=t2.t[:, 0:w], in_=t2.t[:, 0:w], func=AF.Identity,
                                                                    bias=P[B].t[:, v, dc:dc + 1], scale=1.0),
                     reads=[t2.b, P[B].b], writes=[t2.b])
                S.op("vector", lambda e, dc=dc, t2=t2: e.tensor_copy(out=hT.t[:, dc, 0:w], in_=t2.t[:, 0:w]),
                     reads=[t2.b], writes=[hT.b])
                hook(dc, t2)

    def norm_tiles(self, st, sq=None):
        cfg = self.cfg
        return {"sq": sq if sq is not None else self.sb(st, "sq", [128, cfg.DC, cfg.TB], BF16),
                "rr": self.sb(st, "rr", [128, cfg.TB], F32),
                "rinv": self.sb(st, "rinv", [128, cfg.TB], F32),
                "tmpn": [self.sb(st, "tmpn%d" % i, [128, cfg.TB], F32) for i in range(2)]}

    def load_xblk(self, xblk, src, c0, w):
        S = self.S
        DC = self.cfg.DC
        v = src.rearrange("(dc p) t -> p dc t", p=128)
        step = 4
        for d0 in range(0, DC, step):
            d1 = min(DC, d0 + step)
            S.dma("sync", lambda e, d0=d0, d1=d1: e.dma_start(out=xblk.t[:, d0:d1, 0:w], in_=v[:, d0:d1, c0:c0 + w]),
                  writes=[xblk.b], sig=xblk.b)

    def stage_proj(self, l, xin, blocks, own_lat, with_ctx):
        cfg, S, nc, P = self.cfg, self.S, self.nc, self.P
        D, DC, TB = cfg.D, cfg.DC, cfg.TB
        self.begin_stage()
        panels = []
        col = 0
        for (nm, ncols, typ, gi, base) in cfg.segs:
            for p0 in range(0, ncols, 512):
                pw = min(512, ncols - p0)
                panels.append((col + p0, pw, typ, gi, base, p0))
            col += ncols
        with contextlib.ExitStack() as st:
            nt = self.norm_tiles(st)
            xblk = self.sb(st, "xblk", [128, DC, TB], F32)
            hT = self.sb(st, "hT", [128, DC, TB], BF16)
            wsl = [self.sb(st, "wp%d" % i, [128, DC, 512], BF16) for i in range(4)]
            cs = [self.sb(st, "cos%d" % i, [128, TB], F32) for i in range(2)]
            sn = [self.sb(st, "sin%d" % i, [128, TB], F32) for i in range(2)]
            sqh = [self.sb(st, "sqh%d" % i, [128, TB], BF16) for i in range(3)]
            rrh = [self.sb(st, "rrh%d" % i, [128, TB], F32) for i in range(3)]
            rih = [self.sb(st, "rih%d" % i, [128, TB], F32) for i in range(3)]
            qn = [self.sb(st, "qn%d" % i, [128, TB], BF16) for i in range(3)]
            t1 = [self.sb(st, "t1%d" % i, [128, TB], F32) for i in range(3)]
            t2 = [self.sb(st, "t2%d" % i, [128, TB], F32) for i in range(3)]
            qo = [self.sb(st, "qo%d" % i, [128, TB], BF16) for i in range(3)]
            vo = [self.sb(st, "vo%d" % i, [128, 512], BF16) for i in range(3)]
            go = [self.sb(st, "go%d" % i, [128, TB], BF16) for i in range(3)]
            cnt = {"w": 0, "q": 0, "v": 0, "g": 0, "ps": 0}
            qB, qC = [], []

            def step():
                if qC:
                    qC.pop(0)()
                if qB:
                    fb, fc = qB.pop(0)
                    fb()
                    qC.append(fc)

            for bi, (c0, w, is_ctx) in enumerate(blocks):
                own = is_ctx and with_ctx or ((not is_ctx) and c0 < own_lat)
                v = 1 if is_ctx else 0
                self.load_xblk(xblk, xin, c0, w)
                c_t, s_t = cs[bi % 2], sn[bi % 2]
                S.dma("sync", lambda e, c_t=c_t, c0=c0, w=w: e.dma_start(out=c_t.t[:, 0:w], in_=self.cosT[:, c0:c0 + w]), writes=[c_t.b], sig=c_t.b)
                S.dma("sync", lambda e, s_t=s_t, c0=c0, w=w: e.dma_start(out=s_t.t[:, 0:w], in_=self.sinT[:, c0:c0 + w]), writes=[s_t.b], sig=s_t.b)
                self.modnorm(nt, xblk, w, "modA1", "modB1", v, hT)
                for (pc0, pw, typ, gi, base, p0) in panels:
                    if not own and typ not in ("k", "v"):
                        continue
                    wp = wsl[cnt["w"] % len(wsl)]
                    cnt["w"] += 1
                    self.load_panel(wp, self.w_in[l], 0, D, pc0, pw)
                    if typ == "v":
                        for sub in range(w // 128):
                            ps = self.psb(cnt["ps"] % 4)
                            cnt["ps"] += 1
                            for kc in range(DC):
                                S.op("tensor", lambda e, ps=ps, wp=wp, kc=kc, sub=sub, pw=pw: e.matmul(
                                    ps.t[:, 0:pw], lhsT=hT.t[:, kc, sub * 128:(sub + 1) * 128], rhs=wp.t[:, kc, 0:pw],
                                    start=(kc == 0), stop=(kc == DC - 1)), reads=[hT.b, wp.b], writes=[ps.b])
                            o = vo[cnt["v"] % 3]
                            cnt["v"] += 1
                            S.op("scalar", lambda e, o=o, ps=ps, pw=pw: e.activation(out=o.t[:, 0:pw], in_=ps.t[:, 0:pw], func=AF.Copy),
                                 reads=[ps.b], writes=[o.b])
                            kb = (c0 + sub * 128) // 128
                            vb = base + p0
                            if vb < 256:
                                hl = [(vb // 128 + i, 128, i * 128) for i in range(pw // 128)]
                            elif vb < 1280:
                                hl = [(2 + (vb - 256) // 256 + i, 256, i * 256) for i in range(pw // 256)]
                            else:
                                hl = [(6 + (vb - 1280) // 128 + i, 128, i * 128) for i in range(pw // 128)]
                            for (hh, dv, off) in hl:
                                S.dma("sync", lambda e, o=o, hh=hh, dv=dv, off=off, kb=kb: e.dma_start(
                                    out=self.VVh[hh, :, kb * dv:(kb + 1) * dv], in_=o.t[:, off:off + dv]), reads=[o.b], sig=o.b)
                            step()
                        continue
                    for j in range(pw // 128):
                        ps = self.psb(cnt["ps"] % 4)
                        cnt["ps"] += 1
                        for kc in range(DC):
                            S.op("tensor", lambda e, ps=ps, wp=wp, kc=kc, j=j, w=w: e.matmul(
                                ps.t[:, 0:w], lhsT=wp.t[:, kc, j * 128:(j + 1) * 128], rhs=hT.t[:, kc, 0:w],
                                start=(kc == 0), stop=(kc == DC - 1)), reads=[hT.b, wp.b], writes=[ps.b])
                        if typ == "g":
                            o = go[cnt["g"] % 3]
                            cnt["g"] += 1
                            S.op("scalar", lambda e, o=o, ps=ps, w=w: e.activation(out=o.t[:, 0:w], in_=ps.t[:, 0:w], func=AF.Sigmoid),
                                 reads=[ps.b], writes=[o.b])
                            r0 = base + p0 + j * 128
                            S.dma("sync", lambda e, o=o, r0=r0, c0=c0, w=w: e.dma_start(
                                out=self.GT[r0:r0 + 128, c0:c0 + w], in_=o.t[:, 0:w]), reads=[o.b], sig=o.b)
                            step()
                            continue
                        i2 = cnt["q"] % 3
                        cnt["q"] += 1
                        hidx = base + (p0 // 128) + j
                        ps2 = self.psb(4 + (cnt["q"] % 2))
                        ps3 = self.psb(6 + (cnt["q"] % 2))
                        S.op("scalar", lambda e, i2=i2, ps=ps, w=w: e.activation(out=sqh[i2].t[:, 0:w], in_=ps.t[:, 0:w], func=AF.Square),
                             reads=[ps.b], writes=[sqh[i2].b])

                        def phaseB(i2=i2, ps=ps, ps2=ps2, gi=gi, w=w):
                            S.op("tensor", lambda e: e.matmul(ps2.t[:, 0:w], lhsT=P["ones_b"].t[:], rhs=sqh[i2].t[:, 0:w], start=True, stop=True),
                                 reads=[P["ones_b"].b, sqh[i2].b], writes=[ps2.b])
                            S.op("scalar", lambda e: e.activation(out=rrh[i2].t[:, 0:w], in_=ps2.t[:, 0:w], func=AF.Sqrt,
                                                                  bias=P["epsc"].t[:, 0:1], scale=1.0 / 128),
                                 reads=[ps2.b, P["epsc"].b], writes=[rrh[i2].b])
                            S.op("vector", lambda e: e.reciprocal(out=rih[i2].t[:, 0:w], in_=rrh[i2].t[:, 0:w]),
                                 reads=[rrh[i2].b], writes=[rih[i2].b])
                            S.op("vector", lambda e: e.scalar_tensor_tensor(
                                out=qn[i2].t[:, 0:w], in0=ps.t[:, 0:w], scalar=P["gainT"].t[:, gi:gi + 1], in1=rih[i2].t[:, 0:w],
                                op0=ALU.mult, op1=ALU.mult), reads=[ps.b, P["gainT"].b, rih[i2].b], writes=[qn[i2].b])

                        def phaseC(i2=i2, ps3=ps3, w=w, c_t=c_t, s_t=s_t, typ=typ, hidx=hidx, c0=c0):
                            S.op("tensor", lambda e: e.matmul(ps3.t[:, 0:w], lhsT=P["prot_b"].t[:], rhs=qn[i2].t[:, 0:w], start=True, stop=True),
                                 reads=[P["prot_b"].b, qn[i2].b], writes=[ps3.b])
                            S.op("vector", lambda e: e.tensor_tensor(out=t1[i2].t[:, 0:w], in0=qn[i2].t[:, 0:w], in1=c_t.t[:, 0:w], op=ALU.mult),
                                 reads=[qn[i2].b, c_t.b], writes=[t1[i2].b])
                            S.op("vector", lambda e: e.tensor_tensor(out=t2[i2].t[:, 0:w], in0=ps3.t[:, 0:w], in1=s_t.t[:, 0:w], op=ALU.mult),
                                 reads=[ps3.b, s_t.b], writes=[t2[i2].b])
                            o = qo[cnt["g"] % 3]
                            cnt["g"] += 1
                            S.op("vector", lambda e: e.tensor_tensor(out=o.t[:, 0:w], in0=t1[i2].t[:, 0:w], in1=t2[i2].t[:, 0:w], op=ALU.add),
                                 reads=[t1[i2].b, t2[i2].b], writes=[o.b])
                            dst = self.QT if typ == "q" else self.KT
                            S.dma("sync", lambda e: e.dma_start(out=dst[hidx, :, c0:c0 + w], in_=o.t[:, 0:w]), reads=[o.b], sig=o.b)

                        step()
                        qB.append((phaseB, phaseC))
                while qB or qC:
                    step()
            S.flush()

    def attn_head(self, A, qt, w, kt, kcols, vb, vblks, dvc, negb_ap, negb_buf, masks, extra, out_fn, pset):
        S, P = self.S, self.P
        psS = [self.psb(0), self.psb(1)]
        psO = [self.psb(2 + 3 * pset), self.psb(3 + 3 * pset)]
        psZ = self.psb(4 + 3 * pset)
        E = A["E"]
        nkb = len(kcols)
        scale = 1.0 / math.sqrt(128.0)
        HKB = (self.cfg.NT // 128) // 2

        def emit_s(i):
            kc0 = kcols[i]
            kbuf = kt[1][0 if kc0 < HKB * 128 else 1]
            S.op("tensor", lambda e, i=i, kc0=kc0: e.matmul(psS[i % 2].t[:, 0:w], lhsT=kt[0][:, kc0:kc0 + 128], rhs=qt.t[:, 0:w],
                                                            start=True, stop=True),
                 reads=[kbuf, qt.b], writes=[psS[i % 2].b])

        emit_s(0)
        for i in range(nkb):
            if i + 1 < nkb:
                emit_s(i + 1)
            Ei = E[A["ecnt"] % len(E)]
            A["ecnt"] += 1
            S.op("scalar", lambda e, i=i, Ei=Ei: e.activation(out=Ei.t[:, 0:w], in_=psS[i % 2].t[:, 0:w], func=AF.Exp,
                                                               bias=negb_ap, scale=scale),
                 reads=[psS[i % 2].b, negb_buf], writes=[Ei.b])
            if masks is not None and masks[i] is not None:
                m = masks[i]
                S.op("vector", lambda e, Ei=Ei, m=m: e.tensor_tensor(out=Ei.t[:, 0:w], in0=Ei.t[:, 0:w], in1=m.t[:, 0:w], op=ALU.mult),
                     reads=[Ei.b, m.b], writes=[Ei.b])
            S.op("tensor", lambda e, i=i, Ei=Ei: e.matmul(psZ.t[:, 0:w], lhsT=P["ones_b"].t[:], rhs=Ei.t[:, 0:w],
                                                          start=(i == 0), stop=(i == nkb - 1)),
                 reads=[P["ones_b"].b, Ei.b], writes=[psZ.b])
            for c in range(dvc):
                S.op("tensor", lambda e, i=i, Ei=Ei, c=c: e.matmul(psO[c].t[:, 0:w], lhsT=vb[0](vblks[i], c),
                                                                   rhs=Ei.t[:, 0:w], start=(i == 0), stop=(i == nkb - 1)),
                     reads=[vb[1][0 if vblks[i] < HKB else 1], Ei.b], writes=[psO[c].b])
        rz = A["rz"][A["rcnt"] % 2]
        A["rcnt"] += 1
        if extra is not None:
            S.op("vector", lambda e: e.tensor_scalar(out=rz.t[:, 0:w], in0=psZ.t[:, 0:w], scalar1=extra[0], scalar2=None, op0=ALU.add),
                 reads=[psZ.b, extra[1]], writes=[rz.b])
            S.op("vector", lambda e: e.reciprocal(out=rz.t[:, 0:w], in_=rz.t[:, 0:w]), reads=[rz.b], writes=[rz.b])
        else:
            S.op("vector", lambda e: e.reciprocal(out=rz.t[:, 0:w], in_=psZ.t[:, 0:w]), reads=[psZ.b], writes=[rz.b])
        for c in range(dvc):
            out_fn(c, psO[c], rz)

    def stage_attn(self, l, xin, own_blocks):
        cfg, S, nc, P = self.cfg, self.S, self.nc, self.P
        D, DC, TB, NT, Sq = cfg.D, cfg.DC, cfg.TB, cfg.NT, cfg.S
        NKB = NT // 128
        self.begin_stage()
        with contextlib.ExitStack() as st:
            A = {"E": [self.sb(st, "E%d" % i, [128, TB], BF16) for i in range(4)], "ecnt": 0,
                 "rz": [self.sb(st, "rz%d" % i, [128, TB], F32) for i in range(2)], "rcnt": 0}
            ktb = [self.sb(st, "kt%d" % i, [128, NT], BF16) for i in range(1)]
            vbuf = self.sb(st, "vbuf", [128, NKB, 256], BF16)
            HKB = NKB // 2
            kt_hb = [ktb[0].b, S.buf("kt_h1")]
            v_hb = [vbuf.b, S.buf("v_h1")]
            qtl = [self.sb(st, "qt%d" % i, [128, TB], BF16) for i in range(3)]
            oT = [self.sb(st, "oT%d" % i, [128, 8, TB], BF16) for i in range(3)]
            yT = self.sb(st, "yT", [128, DC, TB], BF16)
            wsl = [self.sb(st, "wq%d" % i, [128, 16, 512], BF16) for i in range(2)]
            posq = self.sb(st, "posq", [128, TB], F32)
            posk = self.sb(st, "posk", [128, NKB], F32)
            nband = TB // 128 + 2
            mk = [self.sb(st, "mk%d" % i, [128, TB], BF16) for i in range(nband)]
            dtmp = self.sb(st, "dtmp", [128, TB], F32)
            o12 = [self.sb(st, "o12_%d" % i, [128, 2, TB], F32) for i in range(2)]
            obd = self.sb(st, "obd", [128, 2, TB], F32)
            obq = self.sb(st, "obq", [128, 2, TB], BF16)
            rrb = self.sb(st, "rrb", [128, TB], F32)
            rib = self.sb(st, "rib", [128, TB], F32)
            gt = [self.sb(st, "gt%d" % i, [128, TB], BF16) for i in range(3)]
            tm = [self.sb(st, "tm%d" % i, [128, TB], F32) for i in range(3)]
            sm = [self.sb(st, "sm%d" % i, [128, TB], F32) for i in range(2)]
            xc = [self.sb(st, "xc%d" % i, [128, TB], F32) for i in range(2)]
            xo = [self.sb(st, "xo%d" % i, [128, TB], F32) for i in range(2)]
            S.dma("sync", lambda e: e.dma_start(out=posk.t[:], in_=self.poscol), writes=[posk.b], sig=posk.b)
            cnt = {"kt": 0, "q": 0, "w": 0, "g": 0, "x": 0, "pset": 0}
            ctx_kb = list(range(Sq // 128, NT // 128))

            def load_kt(h):
                k = ktb[0]
                for hf, (a0, a1) in enumerate([(0, HKB * 128), (HKB * 128, NT)]):
                    S.dma("sync", lambda e, k=k, a0=a0, a1=a1, h=h: e.dma_start(out=k.t[:, a0:a1], in_=self.KT[h, :, a0:a1]),
                          writes=[kt_hb[hf]], sig=kt_hb[hf])
                return (k.t, kt_hb)

            vflat = vbuf.t[:].rearrange("p k c -> p (k c)")

            def vbase(dv, kb):
                if dv == 256:
                    return kb * 256
                return kb * 128 if kb < HKB else NKB * 128 + (kb - HKB) * 128

            def load_v(hh, dv, kbs):
                runs = []
                for kb in kbs:
                    if runs and runs[-1][1] == kb and kb != HKB:
                        runs[-1][1] = kb + 1
                    else:
                        runs.append([kb, kb + 1])
                for (a, b) in runs:
                    hf = 0 if a < HKB else 1
                    d0 = vbase(dv, a)
                    S.dma("sync", lambda e, a=a, b=b, dv=dv, hh=hh, d0=d0: e.dma_start(
                        out=vflat[:, d0:d0 + (b - a) * dv], in_=self.VVh[hh, :, a * dv:b * dv]),
                          writes=[v_hb[hf]], sig=v_hb[hf])
                return ((lambda kb, c, dv=dv: vflat[:, vbase(dv, kb) + c * 128: vbase(dv, kb) + (c + 1) * 128]), v_hb)

            def load_q(h, c0, w):
                q = qtl[cnt["q"] % 3]
                cnt["q"] += 1
                S.dma("sync", lambda e, q=q, h=h, c0=c0, w=w: e.dma_start(out=q.t[:, 0:w], in_=self.QT[h, :, c0:c0 + w]),
                      writes=[q.b], sig=q.b)
                return q

            def do_block(c0, w, is_ctx):
                v = 1 if is_ctx else 0
                allkb = ctx_kb if is_ctx else list(range(NKB))
                if is_ctx:
                    a_kb = ctx_kb
                    a_masks = None
                else:
                    band = [((c0 - 128 + 128 * j) % Sq) // 128 for j in range(w // 128 + 2)]
                    a_kb = ctx_kb + band
                    S.dma("sync", lambda e, c0=c0, w=w: e.dma_start(
                        out=posq.t[:, 0:w], in_=self.posrow[0:1, c0:c0 + w].partition_broadcast(128).rearrange("p a b -> p (a b)")),
                          writes=[posq.b], sig=posq.b)
                    a_masks = [None] * len(ctx_kb)
                    for j, kb in enumerate(band):
                        S.op("vector", lambda e, kb=kb, w=w: e.tensor_scalar(out=dtmp.t[:, 0:w], in0=posq.t[:, 0:w], scalar1=posk.t[:, kb:kb + 1],
                                                                             scalar2=None, op0=ALU.subtract),
                             reads=[posq.b, posk.b], writes=[dtmp.b])
                        S.op("vector", lambda e, w=w: e.tensor_tensor(out=dtmp.t[:, 0:w], in0=dtmp.t[:, 0:w], in1=dtmp.t[:, 0:w], op=ALU.mult),
                             reads=[dtmp.b], writes=[dtmp.b])
                        S.op("vector", lambda e, j=j, w=w: e.tensor_scalar(out=mk[j].t[:, 0:w], in0=dtmp.t[:, 0:w], scalar1=16384.5, scalar2=None,
                                                                           op0=ALU.is_le), reads=[dtmp.b], writes=[mk[j].b])
                        a_masks.append(mk[j])
                for kvh in range(2):
                    kt = load_kt(kvh)
                    vv = load_v(kvh, 128, sorted(set(a_kb)))
                    for g in range(4):
                        hq = kvh * 4 + g
                        q = load_q(hq, c0, w)
                        pset = cnt["pset"] % 2
                        cnt["pset"] += 1

                        def fin(c, pso, rz, hq=hq):
                            S.op("vector", lambda e: e.tensor_tensor(out=oT[0].t[:, hq, 0:w], in0=pso.t[:, 0:w], in1=rz.t[:, 0:w], op=ALU.mult),
                                 reads=[pso.b, rz.b], writes=[oT[0].b])
                        self.attn_head(A, q, w, kt, [kb * 128 for kb in a_kb], vv, a_kb, 1, P["negb"].t[:, 0:1], P["negb"].b,
                                       a_masks, (P["esink"].t[:, hq:hq + 1], P["esink"].b), fin, pset)
                for h in range(4):
                    vv = load_v(2 + h, 256, allkb)
                    for comp in range(2):
                        kt = load_kt(2 + 2 * h + comp)
                        q = load_q(8 + 2 * h + comp, c0, w)
                        pset = cnt["pset"] % 2
                        cnt["pset"] += 1

                        def fin(c, pso, rz, comp=comp):
                            S.op("vector", lambda e: e.tensor_tensor(out=o12[comp].t[:, c, 0:w], in0=pso.t[:, 0:w], in1=rz.t[:, 0:w], op=ALU.mult),
                                 reads=[pso.b, rz.b], writes=[o12[comp].b])
                        self.attn_head(A, q, w, kt, [kb * 128 for kb in allkb], vv, allkb, 2, P["negb"].t[:, 1:2], P["negb"].b,
                                       None, None, fin, pset)
                    S.op("vector", lambda e: e.scalar_tensor_tensor(out=obd.t[:, :, 0:w], in0=o12[1].t[:, :, 0:w], scalar=P["neglam"].t[:, 0:1],
                                                                    in1=o12[0].t[:, :, 0:w], op0=ALU.mult, op1=ALU.add),
                         reads=[o12[0].b, o12[1].b, P["neglam"].b], writes=[obd.b])
                    S.op("scalar", lambda e: e.activation(out=obq.t[:, :, 0:w], in_=obd.t[:, :, 0:w], func=AF.Square), reads=[obd.b], writes=[obq.b])
                    psn = self.psb(7)
                    for c in range(2):
                        S.op("tensor", lambda e, c=c: e.matmul(psn.t[:, 0:w], lhsT=P["ones_b"].t[:], rhs=obq.t[:, c, 0:w], start=(c == 0), stop=(c == 1)),
                             reads=[P["ones_b"].b, obq.b], writes=[psn.b])
                    S.op("scalar", lambda e: e.activation(out=rrb.t[:, 0:w], in_=psn.t[:, 0:w], func=AF.Sqrt, bias=P["epsc"].t[:, 0:1], scale=1.0 / 256),
                         reads=[psn.b, P["epsc"].b], writes=[rrb.b])
                    S.op("vector", lambda e: e.reciprocal(out=rib.t[:, 0:w], in_=rrb.t[:, 0:w]), reads=[rrb.b], writes=[rib.b])
                    for c in range(2):
                        S.op("vector", lambda e, c=c, h=h: e.scalar_tensor_tensor(out=oT[1].t[:, 2 * h + c, 0:w], in0=obd.t[:, c, 0:w],
                                                                                 scalar=P["sgT"].t[:, c:c + 1], in1=rib.t[:, 0:w], op0=ALU.mult, op1=ALU.mult),
                             reads=[obd.b, P["sgT"].b, rib.b], writes=[oT[1].b])
                for kvh in range(2):
                    kt = load_kt(10 + kvh)
                    vv = load_v(6 + kvh, 128, allkb)
                    for g in range(4):
                        hq = kvh * 4 + g
                        q = load_q(16 + hq, c0, w)
                        pset = cnt["pset"] % 2
                        cnt["pset"] += 1

                        def fin(c, pso, rz, hq=hq):
                            S.op("vector", lambda e: e.tensor_tensor(out=oT[2].t[:, hq, 0:w], in0=pso.t[:, 0:w], in1=rz.t[:, 0:w], op=ALU.mult),
                                 reads=[pso.b, rz.b], writes=[oT[2].b])
                        self.attn_head(A, q, w, kt, [kb * 128 for kb in allkb], vv, allkb, 1, P["negb"].t[:, 2:3], P["negb"].b,
                                       None, None, fin, pset)
                for pn in range(D // 512 if D >= 512 else 1):
                    pw = min(512, D)
                    wps = [wsl[0], wsl[0], wsl[1]]
                    koff = [0, 8, 0]
                    for br in range(3):
                        self.load_panel(wps[br], self.w_branch[l, br], 0, 1024, pn * 512, pw, k_off=koff[br])
                    for j in range(pw // 128):
                        n = pn * 4 + j
                        tms = []
                        for br in range(3):
                            ps = self.psb(br + 3 * (n % 2))
                            for kc in range(8):
                                S.op("tensor", lambda e, ps=ps, br=br, kc=kc, j=j: e.matmul(
                                    ps.t[:, 0:w], lhsT=wps[br].t[:, koff[br] + kc, j * 128:(j + 1) * 128], rhs=oT[br].t[:, kc, 0:w],
                                    start=(kc == 0), stop=(kc == 7)), reads=[wps[br].b, oT[br].b], writes=[ps.b])
                            g_t = gt[cnt["g"] % 3]
                            t_t = tm[cnt["g"] % 3]
                            cnt["g"] += 1
                            r0 = br * D + n * 128
                            S.dma("sync", lambda e, g_t=g_t, r0=r0: e.dma_start(out=g_t.t[:, 0:w], in_=self.GT[r0:r0 + 128, c0:c0 + w]),
                                  writes=[g_t.b], sig=g_t.b)
                            S.op("vector", lambda e, ps=ps, g_t=g_t, t_t=t_t: e.tensor_tensor(out=t_t.t[:, 0:w], in0=ps.t[:, 0:w], in1=g_t.t[:, 0:w], op=ALU.mult),
                                 reads=[ps.b, g_t.b], writes=[t_t.b])
                            tms.append(t_t)
                        s_t = sm[n % 2]
                        S.op("gpsimd", lambda e, s_t=s_t, tms=tms: e.tensor_tensor(out=s_t.t[:, 0:w], in0=tms[0].t[:, 0:w], in1=tms[1].t[:, 0:w], op=ALU.add),
                             reads=[tms[0].b, tms[1].b], writes=[s_t.b])
                        S.op("gpsimd", lambda e, s_t=s_t, tms=tms, n=n: e.tensor_tensor(out=yT.t[:, n, 0:w], in0=s_t.t[:, 0:w], in1=tms[2].t[:, 0:w], op=ALU.add),
                             reads=[s_t.b, tms[2].b], writes=[yT.b])
                for pn in range(D // 512 if D >= 512 else 1):
                    pw = min(512, D)
                    wp = wsl[cnt["w"] % 2]
                    cnt["w"] += 1
                    self.load_panel(wp, self.w_out[l], 0, D, pn * 512, pw)
                    for j in range(pw // 128):
                        n = pn * 4 + j
                        ps = self.psb(6 + (n % 2))
                        for kc in range(DC):
                            S.op("tensor", lambda e, ps=ps, wp=wp, kc=kc, j=j: e.matmul(
                                ps.t[:, 0:w], lhsT=wp.t[:, kc, j * 128:(j + 1) * 128], rhs=yT.t[:, kc, 0:w],
                                start=(kc == 0), stop=(kc == DC - 1)), reads=[wp.b, yT.b], writes=[ps.b])
                        x_t = xc[cnt["x"] % 2]
                        o_t = xo[cnt["x"] % 2]
                        cnt["x"] += 1
                        S.dma("sync", lambda e, x_t=x_t, n=n: e.dma_start(out=x_t.t[:, 0:w], in_=xin[n * 128:(n + 1) * 128, c0:c0 + w]),
                              writes=[x_t.b], sig=x_t.b)
                        S.op("vector", lambda e, ps=ps, x_t=x_t, o_t=o_t, n=n, v=v: e.scalar_tensor_tensor(
                            out=o_t.t[:, 0:w], in0=ps.t[:, 0:w], scalar=P["modG1"].t[:, v, n:n + 1], in1=x_t.t[:, 0:w],
                            op0=ALU.mult, op1=ALU.add), reads=[ps.b, x_t.b, P["modG1"].b], writes=[o_t.b])
                        S.dma("sync", lambda e, o_t=o_t, n=n: e.dma_start(out=self.X1T[n * 128:(n + 1) * 128, c0:c0 + w], in_=o_t.t[:, 0:w]),
                              reads=[o_t.b], sig=o_t.b)
            for blk in own_blocks:
                do_block(*blk)
            S.flush()

    def stage_ffn(self, l, own_blocks, xout):
        cfg, S, nc, P = self.cfg, self.S, self.nc, self.P
        D, DC, TB, NE = cfg.D, cfg.DC, cfg.TB, cfg.NE
        moe = (self.lsem % 2 == 1)
        li = 0
        DFF = cfg.DFFE if moe else cfg.DFF
        NF = DFF // 128
        self.begin_stage()
        with contextlib.ExitStack() as st:
            xa = self.sb(st, "xa", [128, DC, TB], F32)
            h2 = self.sb(st, "h2", [128, DC, TB], BF16)
            u = self.sb(st, "u", [128, max(NF, DC), TB], BF16)
            nt = self.norm_tiles(st, sq=u)
            wsl = [self.sb(st, "wf%d" % i, [128, 16, 512], BF16) for i in range(3 if moe else 5)]
            sa = [self.sb(st, "sa%d" % i, [128, TB], F32) for i in range(2)]
            xc = [self.sb(st, "xc%d" % i, [128, TB], F32) for i in range(1)]
            xo = [self.sb(st, "xo%d" % i, [128, TB], F32) for i in range(1)]
            tw = [self.sb(st, "tw%d" % i, [128, TB], F32) for i in range(1)]
            cnt = {"w": 0, "ab": 0, "x": 0, "t": 0}
            if moe:
                wr = self.sb(st, "wr", [128, DC, NE], F32)
                S.dma("sync", lambda e: e.dma_start(out=wr.t[:], in_=self.moe_router[li].rearrange("(dc p) e -> p dc e", p=128)),
                      writes=[wr.b], sig=wr.b)
                lgT = self.sb(st, "lgT", [NE, TB], F32)
                lg = self.sb(st, "lg", [128, NE], F32)
                m8 = self.sb(st, "m8", [128, 8], F32)
                msk = self.sb(st, "msk", [128, NE], F32)
                ex = self.sb(st, "ex", [128, NE], F32)
                nv1 = self.sb(st, "nv1", [128, 1], F32)
                den = self.sb(st, "den", [128, 1], F32)
                wt = self.sb(st, "wt", [128, NE], F32)
                wtb = self.sb(st, "wtb", [128, 128], F32)
                wtbc = self.sb(st, "wtbc", [128, NE, TB], BF16)

            def ffn_expert(w, w1, w3, w2, e_idx, final_out):
                for fp in range(0, DFF, 512):
                    pw = min(512, DFF - fp)
                    wa = wsl[cnt["w"] % len(wsl)]
                    cnt["w"] += 1
                    wb = wsl[cnt["w"] % len(wsl)]
                    cnt["w"] += 1
                    self.load_panel(wa, w1, 0, D, fp, pw)
                    self.load_panel(wb, w3, 0, D, fp, pw)
                    for j in range(pw // 128):
                        f = fp // 128 + j
                        pa = self.psb(cnt["ab"] % 2)
                        pb = self.psb(2 + cnt["ab"] % 2)
                        s_t = sa[cnt["ab"] % 2]
                        cnt["ab"] += 1
                        for kc in range(DC):
                            S.op("tensor", lambda e, pa=pa, wa=wa, kc=kc, j=j: e.matmul(
                                pa.t[:, 0:w], lhsT=wa.t[:, kc, j * 128:(j + 1) * 128], rhs=h2.t[:, kc, 0:w],
                                start=(kc == 0), stop=(kc == DC - 1)), reads=[wa.b, h2.b], writes=[pa.b])
                        for kc in range(DC):
                            S.op("tensor", lambda e, pb=pb, wb=wb, kc=kc, j=j: e.matmul(
                                pb.t[:, 0:w], lhsT=wb.t[:, kc, j * 128:(j + 1) * 128], rhs=h2.t[:, kc, 0:w],
                                start=(kc == 0), stop=(kc == DC - 1)), reads=[wb.b, h2.b], writes=[pb.b])
                        S.op("scalar", lambda e, pa=pa, s_t=s_t: e.activation(out=s_t.t[:, 0:w], in_=pa.t[:, 0:w], func=AF.Silu),
                             reads=[pa.b], writes=[s_t.b])
                        S.op("vector", lambda e, pb=pb, s_t=s_t, f=f: e.tensor_tensor(out=u.t[:, f, 0:w], in0=pb.t[:, 0:w], in1=s_t.t[:, 0:w], op=ALU.mult),
                             reads=[pb.b, s_t.b], writes=[u.b])
                for ng in range(0, D, 512):
                    pw = min(512, D - ng)
                    nj = pw // 128
                    pso = [self.psb(4 + j) for j in range(nj)]
                    for f0 in range(0, NF, 16):
                        f1 = min(NF, f0 + 16)
                        wp = wsl[cnt["w"] % len(wsl)]
                        cnt["w"] += 1
                        self.load_panel(wp, w2, f0 * 128, (f1 - f0) * 128, ng, pw)
                        for fi in range(f0, f1):
                            for j in range(nj):
                                S.op("tensor", lambda e, wp=wp, fi=fi, f0=f0, j=j: e.matmul(
                                    pso[j].t[:, 0:w], lhsT=wp.t[:, fi - f0, j * 128:(j + 1) * 128], rhs=u.t[:, fi, 0:w],
                                    start=(fi == 0), stop=(fi == NF - 1)), reads=[wp.b, u.b], writes=[pso[j].b])
                    for j in range(nj):
                        n = ng // 128 + j
                        if e_idx is None:
                            final_out(n, pso[j], True)
                        elif e_idx == 0:
                            S.op("vector", lambda e, j=j, n=n: e.tensor_tensor(out=xa.t[:, n, 0:w], in0=pso[j].t[:, 0:w], in1=wtbc.t[:, 0, 0:w], op=ALU.mult),
                                 reads=[pso[j].b, wtbc.b], writes=[xa.b])
                        else:
                            t_t = tw[0]
                            cnt["t"] += 1
                            S.op("vector", lambda e, j=j, t_t=t_t, e_idx=e_idx: e.tensor_tensor(out=t_t.t[:, 0:w], in0=pso[j].t[:, 0:w], in1=wtbc.t[:, e_idx, 0:w], op=ALU.mult),
                                 reads=[pso[j].b, wtbc.b], writes=[t_t.b])
                            S.op("gpsimd", lambda e, n=n, t_t=t_t: e.tensor_tensor(out=xa.t[:, n, 0:w], in0=xa.t[:, n, 0:w], in1=t_t.t[:, 0:w], op=ALU.add),
                                 reads=[xa.b, t_t.b], writes=[xa.b])

            def do_block(c0, w, is_ctx):
                v = 1 if is_ctx else 0

                def final_out(n, src, src_is_psum, c0=c0, w=w, v=v):
                    x_t = xc[0]
                    o_t = xo[0]
                    cnt["x"] += 1
                    S.dma("sync", lambda e: e.dma_start(out=x_t.t[:, 0:w], in_=self.X1T[n * 128:(n + 1) * 128, c0:c0 + w]),
                          writes=[x_t.b], sig=x_t.b)
                    if src_is_psum:
                        S.op("vector", lambda e: e.scalar_tensor_tensor(out=o_t.t[:, 0:w], in0=src.t[:, 0:w], scalar=P["modG2"].t[:, v, n:n + 1],
                                                                        in1=x_t.t[:, 0:w], op0=ALU.mult, op1=ALU.add),
                             reads=[src.b, x_t.b, P["modG2"].b], writes=[o_t.b])
                    else:
                        S.op("vector", lambda e: e.scalar_tensor_tensor(out=o_t.t[:, 0:w], in0=src.t[:, n, 0:w], scalar=P["modG2"].t[:, v, n:n + 1],
                                                                        in1=x_t.t[:, 0:w], op0=ALU.mult, op1=ALU.add),
                             reads=[src.b, x_t.b, P["modG2"].b], writes=[o_t.b])
                    oc0 = self.xmap(c0)
                    S.dma("sync", lambda e: e.dma_start(out=xout[n * 128:(n + 1) * 128, oc0:oc0 + w], in_=o_t.t[:, 0:w]),
                          reads=[o_t.b], sig=o_t.b)

                self.load_xblk(xa, self.X1T, c0, w)
                if not moe:
                    self.modnorm(nt, xa, w, "modA2", "modB2", v, h2)
                    ffn_expert(w, self.dense_w1[li], self.dense_w3[li], self.dense_w2[li], None, final_out)
                    return
                psr = self.psb(6)

                def hook(dc, t2):
                    S.op("tensor", lambda e: e.matmul(psr.t[0:NE, 0:w], lhsT=wr.t[:, dc, :], rhs=t2.t[:, 0:w], start=(dc == 0), stop=(dc == DC - 1)),
                         reads=[wr.b, t2.b], writes=[psr.b])
                self.modnorm(nt, xa, w, "modA2", "modB2", v, h2, hook=hook)
                S.op("vector", lambda e: e.tensor_copy(out=lgT.t[:, 0:w], in_=psr.t[0:NE, 0:w]), reads=[psr.b], writes=[lgT.b])
                for sub in range(w // 128):
                    pst = self.psb(5)
                    S.op("tensor", lambda e, sub=sub: e.matmul(pst.t[:, 0:NE], lhsT=lgT.t[0:NE, sub * 128:(sub + 1) * 128],
                                                               rhs=P["ident_f"].t[0:NE, 0:NE], start=True, stop=True),
                         reads=[lgT.b, P["ident_f"].b], writes=[pst.b])
                    S.op("vector", lambda e: e.tensor_copy(out=lg.t[:], in_=pst.t[:, 0:NE]), reads=[pst.b], writes=[lg.b])
                    S.op("vector", lambda e: e.max(out=m8.t[:], in_=lg.t[:]), reads=[lg.b], writes=[m8.b])
                    S.op("vector", lambda e: e.tensor_scalar(out=msk.t[:], in0=lg.t[:], scalar1=m8.t[:, 1:2], scalar2=None, op0=ALU.is_ge),
                         reads=[lg.b, m8.b], writes=[msk.b])
                    S.op("vector", lambda e: e.tensor_scalar(out=nv1.t[:], in0=m8.t[:, 0:1], scalar1=-1.0, scalar2=None, op0=ALU.mult),
                         reads=[m8.b], writes=[nv1.b])
                    S.op("scalar", lambda e: e.activation(out=ex.t[:], in_=lg.t[:], func=AF.Exp, bias=nv1.t[:, 0:1], scale=1.0),
                         reads=[lg.b, nv1.b], writes=[ex.b])
                    S.op("scalar", lambda e: e.activation(out=den.t[:], in_=m8.t[:, 1:2], func=AF.Exp, bias=nv1.t[:, 0:1], scale=1.0),
                         reads=[m8.b, nv1.b], writes=[den.b])
                    S.op("vector", lambda e: e.tensor_scalar(out=den.t[:], in0=den.t[:], scalar1=1.0, scalar2=None, op0=ALU.add),
                         reads=[den.b], writes=[den.b])
                    S.op("vector", lambda e: e.reciprocal(out=den.t[:], in_=den.t[:]), reads=[den.b], writes=[den.b])
                    S.op("vector", lambda e: e.scalar_tensor_tensor(out=wt.t[:], in0=ex.t[:], scalar=den.t[:, 0:1], in1=msk.t[:], op0=ALU.mult, op1=ALU.mult),
                         reads=[ex.b, den.b, msk.b], writes=[wt.b])
                    for ei in range(NE):
                        S.op("vector", lambda e, ei=ei: e.tensor_scalar(out=wtb.t[:], in0=P["ones_f"].t[:], scalar1=wt.t[:, ei:ei + 1], scalar2=None, op0=ALU.mult),
                             reads=[wt.b, P["ones_f"].b], writes=[wtb.b])
                        psb_ = self.psb(4)
                        S.op("tensor", lambda e, psb_=psb_: e.matmul(psb_.t[:, 0:128], lhsT=wtb.t[:], rhs=P["ident_f"].t[:], start=True, stop=True),
                             reads=[wtb.b, P["ident_f"].b], writes=[psb_.b])
                        S.op("vector", lambda e, ei=ei, sub=sub, psb_=psb_: e.tensor_copy(out=wtbc.t[:, ei, sub * 128:(sub + 1) * 128], in_=psb_.t[:, 0:128]),
                             reads=[psb_.b], writes=[wtbc.b])
                for ei in range(NE):
                    ffn_expert(w, self.moe_w1[ei], self.moe_w3[ei], self.moe_w2[ei], ei, final_out)
                for n in range(DC):
                    final_out(n, xa, False)
            for blk in own_blocks:
                do_block(*blk)
            S.flush()


def make_consts():
    c = np.zeros((128, 384), np.float32)
    c[:, 0:128] = np.eye(128, dtype=np.float32)
    prot = np.zeros((128, 128), np.float32)
    for d in range(128):
        partner = d + 32 if (d % 64) < 32 else d - 32
        prot[partner, d] = 1.0
    c[:, 128:256] = prot
    c[:, 256:384] = 1.0
    return c


def core_inputs(cfg, inputs, core, shared):
    b, q = core // 4, core % 4
    S, CTX, NT, OWN = cfg.S, cfg.CTX, cfg.NT, cfg.OWN
    x = np.asarray(inputs["x"])[b]
    ctx = np.asarray(inputs["ctx"])[b]
    tok = (q * OWN + np.arange(S)) % S
    xT = np.ascontiguousarray(np.concatenate([x[tok].T, ctx.T], axis=1), dtype=np.float32)
    row = (tok // cfg.GRID_W).astype(np.float32)
    colp = (tok % cfg.GRID_W).astype(np.float32)
    inv = (10000.0 ** (-np.arange(32, dtype=np.float32) / 32)).astype(np.float32)
    cosT = np.ones((128, NT), np.float32)
    sinT = np.zeros((128, NT), np.float32)
    for a, pos in enumerate([row, colp]):
        ang = (pos[None, :] * inv[:, None]).astype(np.float32)
        c, s = np.cos(ang).astype(np.float32), np.sin(ang).astype(np.float32)
        cosT[a * 64:a * 64 + 32, :S] = c
        cosT[a * 64 + 32:a * 64 + 64, :S] = c
        sinT[a * 64:a * 64 + 32, :S] = -s
        sinT[a * 64 + 32:a * 64 + 64, :S] = s
    pos = np.concatenate([tok.astype(np.float32), np.full((CTX,), -1.0e6, np.float32)])
    m = {
        "xT": xT, "cosT": cosT, "sinT": sinT, "posrow": pos[None, :].copy(),
        "poscol": np.ascontiguousarray(pos.reshape(NT // 128, 128).T),
        "consts": shared["consts"],
        "cvec": np.ascontiguousarray(np.stack([np.asarray(inputs["c"])[b], np.asarray(inputs["c_ctx"])], axis=0), dtype=np.float32),
    }
    m.update(shared["w"])
    return m


def tile_w(W):
    W = np.asarray(W, dtype=np.float32)
    K, N = W.shape[-2:]
    lead = W.shape[:-2]
    NP = (N + 511) // 512
    if NP * 512 != N:
        Wp = np.zeros(lead + (K, NP * 512), np.float32)
        Wp[..., :N] = W
        W = Wp
    W = W.reshape(lead + (K // 128, 128, NP, 512))
    nl = len(lead)
    perm = tuple(range(nl)) + (nl + 2, nl + 1, nl + 0, nl + 3)
    return np.ascontiguousarray(W.transpose(perm))


def shared_inputs(cfg, inputs, lw):
    def lay(a):
        a = np.asarray(a, dtype=np.float32)
        return a if lw is None else a[lw:lw + 1]
    w = {}
    for k in ["b_mod", "norm1", "norm2", "a_sink", "b_subln"]:
        w[k] = lay(inputs[k])
    for k in ["w_mod", "w_in", "w_branch", "w_out"]:
        w[k] = tile_w(lay(inputs[k]))
    L = w["b_mod"].shape[0]
    w["qk_gain"] = lay(inputs["qk_gain"]).reshape(L, 6 * 128)
    w["b_lambda"] = lay(inputs["b_lambda"]).reshape(L, 4 * 128)
    need_dense = lw in (None, 0)
    need_moe = lw in (None, 1)
    for k in ["dense_w1", "dense_w3", "dense_w2"]:
        w[k] = tile_w(inputs[k]) if need_dense else np.zeros((1, 1, 128, 1, 512), np.float32)
    w["moe_router"] = np.asarray(inputs["moe_router"], dtype=np.float32)
    for k in ["moe_w1", "moe_w3", "moe_w2"]:
        if need_moe:
            a = np.asarray(inputs[k], dtype=np.float32)
            a = a.reshape(a.shape[1:]) if a.shape[0] == 1 else a
            w[k] = tile_w(a)
        else:
            w[k] = np.zeros((cfg.NE, 1, 128, 1, 512), np.float32)
    return {"consts": make_consts(), "w": w}


_NC_CACHE = {}


def _program(cfg, debug=False):
    key = (cfg.D, cfg.S, cfg.DFF, cfg.DFFE, cfg.TB, cfg.DEPTH, cfg.mode, debug)
    if key not in _NC_CACHE:
        bld = Builder(cfg)
        bld.debug = debug
        _NC_CACHE[key] = bld.build()
    return _NC_CACHE[key]


def run(cfg, inputs, trace=False, debug=False):
    nc = _program(cfg, debug)
    sh = shared_inputs(cfg, inputs, None)
    in_maps = [core_inputs(cfg, inputs, c, sh) for c in range(8)]
    res = run_bass_kernel_spmd(nc, in_maps, core_ids=list(range(8)), **({"trace": True} if trace else {}))
    B = np.asarray(inputs["x"]).shape[0]
    out = np.zeros((B, cfg.S, cfg.D), np.float32)
    for c in range(8):
        b, q = c // 4, c % 4
        out[b, q * cfg.OWN:(q + 1) * cfg.OWN, :] = res.results[c]["yT"].T
    return out, res


def run_split(inputs, cfg_kw=None, trace=False):
    cfg_kw = cfg_kw or {}
    cfg0 = Cfg(mode="L0", **cfg_kw)
    nc0 = _program(cfg0)
    sh0 = shared_inputs(cfg0, inputs, 0)
    tk = {"trace": True} if trace else {}
    res0 = run_bass_kernel_spmd(nc0, [core_inputs(cfg0, inputs, c, sh0) for c in range(8)], core_ids=list(range(8)), **tk)
    x1 = np.zeros(np.asarray(inputs["x"]).shape, np.float32)
    ctx1 = np.zeros(np.asarray(inputs["ctx"]).shape, np.float32)
    OWN = cfg0.OWN
    for c in range(8):
        b, q = c // 4, c % 4
        y = res0.results[c]["yT"]
        x1[b, q * OWN:(q + 1) * OWN, :] = y[:, :OWN].T
        if q == 0:
            ctx1[b] = y[:, OWN:].T
    inputs1 = dict(inputs)
    inputs1["x"], inputs1["ctx"] = x1, ctx1
    cfg1 = Cfg(mode="L1", **cfg_kw)
    nc1 = _program(cfg1)
    sh1 = shared_inputs(cfg1, inputs, 1)
    res1 = run_bass_kernel_spmd(nc1, [core_inputs(cfg1, inputs1, c, sh1) for c in range(8)], core_ids=list(range(8)), **tk)
    out = np.zeros(x1.shape, np.float32)
    for c in range(8):
        b, q = c // 4, c % 4
        out[b, q * OWN:(q + 1) * OWN, :] = res1.results[c]["yT"].T
    return out, (res0, res1)


SPLIT = False


def kernel(**inputs):
    if SPLIT:
        out, _ = run_split(inputs)
        return out
    cfg = Cfg()
    out, _ = run(cfg, inputs)
    return out
```
